# Optimizing a Trainium2 kernel written in Bass

```python
import jax, jax.numpy as jnp
from jax import lax
import numpy as np

D_MODEL = 1024
BATCH = 8
SEQ = 4096
DEPTH = 1

POOL_WIDTH = 256
POOL_WINDOWS = (2, 4, 8, 16)
POOL_GROUPS = len(POOL_WINDOWS)
POOL_GROUP_DIM = POOL_WIDTH // POOL_GROUPS
MLA_HEADS = 6
QK_NOPE_DIM = 128
QK_ROPE_DIM = 64
QK_HEAD_DIM = QK_NOPE_DIM + QK_ROPE_DIM
V_HEAD_DIM = 128
Q_LORA_RANK = 512
KV_LORA_RANK = 256
ROPE_THETA = 10000.0
Q_BLOCK = 128
IN_WIDTH = POOL_WIDTH + Q_LORA_RANK + KV_LORA_RANK + QK_ROPE_DIM
MIX_WIDTH = POOL_WIDTH + MLA_HEADS * V_HEAD_DIM
N_GROUPS = 4
EXPERTS_PER_GROUP = 8
N_EXPERTS = N_GROUPS * EXPERTS_PER_GROUP
TOP_K = 2
D_EXPERT = 256
ROW_BLOCK = 128
N_MOD = 6
EPS = 1e-6

kernel_name = "hybrid_pool_mla_hmoe_adaln"


def rmsnorm(x, g):
    xf = x.astype(jnp.float32)
    y = xf * lax.rsqrt(jnp.mean(xf * xf, axis=-1, keepdims=True) + EPS)
    return (y * g.astype(jnp.float32)).astype(x.dtype)


def rope_tables(positions, dim):
    inv_freq = ROPE_THETA ** (-(jnp.arange(0, dim, 2, dtype=jnp.float32) / dim))
    ang = positions.astype(jnp.float32)[..., None] * inv_freq
    return jnp.cos(ang), jnp.sin(ang)


def apply_rope(x, cos, sin):
    xf = x.astype(jnp.float32)
    x1, x2 = jnp.split(xf, 2, axis=-1)
    return jnp.concatenate([x1 * cos - x2 * sin, x2 * cos + x1 * sin], axis=-1).astype(x.dtype)


def pool_mixer(p, w_pool, pool_scale):
    B, S, C = p.shape
    cs = jnp.cumsum(p.astype(jnp.float32), axis=1)
    t = jnp.arange(1, S + 1, dtype=jnp.float32)
    means = []
    for gi, w in enumerate(POOL_WINDOWS):
        cg = cs[..., gi * POOL_GROUP_DIM:(gi + 1) * POOL_GROUP_DIM]
        lagged = jnp.pad(cg, ((0, 0), (w, 0), (0, 0)))[:, :S]
        cnt = jnp.minimum(t, float(w))[None, :, None]
        means.append((cg - lagged) / cnt)
    pooled = jnp.concatenate(means, axis=-1).astype(p.dtype) - p
    y = jnp.einsum('bsgc,gcd->bsgd', pooled.reshape(B, S, POOL_GROUPS, POOL_GROUP_DIM), w_pool)
    return y.reshape(B, S, C) * pool_scale


def mla_attention(c_q, c_kv, k_rope_in, positions, q_norm_g, w_uq, kv_norm_g, w_ukv):
    B, S, _ = c_q.shape
    q = (rmsnorm(c_q, q_norm_g) @ w_uq).reshape(B, S, MLA_HEADS, QK_HEAD_DIM)
    q_nope, q_rope = q[..., :QK_NOPE_DIM], q[..., QK_NOPE_DIM:]
    kv = (rmsnorm(c_kv, kv_norm_g) @ w_ukv).reshape(B, S, MLA_HEADS, QK_NOPE_DIM + V_HEAD_DIM)
    k_nope, v = kv[..., :QK_NOPE_DIM], kv[..., QK_NOPE_DIM:]
    cos, sin = rope_tables(positions, QK_ROPE_DIM)
    q_rope = apply_rope(q_rope, cos[:, :, None, :], sin[:, :, None, :])
    k_rope = apply_rope(k_rope_in, cos, sin)
    scale = QK_HEAD_DIM ** -0.5
    key_idx = jnp.arange(S)

    def block(i):
        start = i * Q_BLOCK
        qn = lax.dynamic_slice_in_dim(q_nope, start, Q_BLOCK, axis=1)
        qr = lax.dynamic_slice_in_dim(q_rope, start, Q_BLOCK, axis=1)
        s = (jnp.einsum('bqhd,bkhd->bhqk', qn, k_nope)
             + jnp.einsum('bqhd,bkd->bhqk', qr, k_rope)).astype(jnp.float32) * scale
        q_idx = start + jnp.arange(Q_BLOCK)
        s = jnp.where(key_idx[None, :] <= q_idx[:, None], s, -jnp.inf)
        pr = jax.nn.softmax(s, axis=-1).astype(v.dtype)
        return jnp.einsum('bhqk,bkhd->bqhd', pr, v)

    out = lax.map(block, jnp.arange(S // Q_BLOCK))
    return out.transpose(1, 0, 2, 3, 4).reshape(B, S, MLA_HEADS * V_HEAD_DIM)


def hierarchical_moe(h, w_group, b_group, w_router, b_router, w_gate_up, w_down):
    B, S, D = h.shape
    N = B * S
    hf = h.reshape(N, D)
    g_logits = (hf @ w_group).astype(jnp.float32)
    g_prob = jax.nn.softmax(g_logits, axis=-1)
    g_sel = jnp.argmax(g_logits + b_group.astype(jnp.float32), axis=-1)
    e_logits = (hf @ w_router).astype(jnp.float32).reshape(N, N_GROUPS, EXPERTS_PER_GROUP)
    e_in = jnp.take_along_axis(e_logits, g_sel[:, None, None], axis=1)[:, 0]
    b_in = b_router.astype(jnp.float32).reshape(N_GROUPS, EXPERTS_PER_GROUP)[g_sel]
    _, local_idx = lax.top_k(e_in + b_in, TOP_K)
    sel_prob = jnp.take_along_axis(jax.nn.softmax(e_in, axis=-1), local_idx, axis=-1)
    sel_prob = sel_prob / jnp.sum(sel_prob, axis=-1, keepdims=True)
    gp = jnp.take_along_axis(g_prob, g_sel[:, None], axis=-1)
    weights = gp * sel_prob
    expert_ids = g_sel[:, None] * EXPERTS_PER_GROUP + local_idx

    P = N * TOP_K
    flat_e = expert_ids.reshape(P).astype(jnp.int32)
    flat_tok = jnp.repeat(jnp.arange(N, dtype=jnp.int32), TOP_K)
    flat_w = weights.reshape(P)
    order = jnp.argsort(flat_e)
    sorted_e = flat_e[order]
    counts = jnp.zeros((N_EXPERTS,), jnp.int32).at[flat_e].add(1)
    padded = ((counts + ROW_BLOCK - 1) // ROW_BLOCK) * ROW_BLOCK
    starts = jnp.cumsum(counts) - counts
    pends = jnp.cumsum(padded)
    pstarts = pends - padded
    dest = pstarts[sorted_e] + (jnp.arange(P, dtype=jnp.int32) - starts[sorted_e])
    P_pad = P + N_EXPERTS * ROW_BLOCK
    n_rb = P_pad // ROW_BLOCK
    row_tok = jnp.full((P_pad,), N, jnp.int32).at[dest].set(flat_tok[order])
    row_w = jnp.zeros((P_pad,), h.dtype).at[dest].set(flat_w[order].astype(h.dtype))
    block_e = jnp.searchsorted(pends, jnp.arange(n_rb, dtype=jnp.int32) * ROW_BLOCK, side='right')
    block_e = jnp.minimum(block_e, N_EXPERTS - 1)
    x_pad = jnp.concatenate([hf, jnp.zeros((1, D), hf.dtype)], axis=0)
    xs = x_pad[row_tok].reshape(n_rb, ROW_BLOCK, D)

    def expert_block(args):
        xb, e = args
        gu = xb @ w_gate_up[e]
        gate, up = gu[:, :D_EXPERT], gu[:, D_EXPERT:]
        return (jax.nn.silu(gate) * up) @ w_down[e]

    ys = lax.map(expert_block, (xs, block_e)).reshape(P_pad, D) * row_w[:, None]
    out = jax.ops.segment_sum(ys, row_tok, num_segments=N + 1)[:N]
    return out.reshape(B, S, D)


def setup_inputs(seed: int = 0) -> dict:
    key = jax.random.key(seed)
    ks = jax.random.split(key, 24)
    L, D = DEPTH, D_MODEL
    f32 = jnp.float32

    def nrm(k, shape, fan_in):
        return jax.random.normal(k, shape, f32) * (fan_in ** -0.5)

    def gain(k, shape):
        return 1.0 + 0.02 * jax.random.normal(k, shape, f32)

    x = jax.random.normal(ks[0], (BATCH, SEQ, D), f32)
    c = jax.random.normal(ks[1], (BATCH, D), f32)
    offsets = jax.random.randint(ks[2], (BATCH, 1), 0, 1024, jnp.int32)
    positions = (offsets + jnp.arange(SEQ, dtype=jnp.int32)[None, :]).astype(jnp.int32)
    return {
        "x": x,
        "c": c,
        "positions": positions,
        "w_mod": nrm(ks[3], (L, D, N_MOD * D), D),
        "b_mod": 0.02 * jax.random.normal(ks[4], (L, N_MOD * D), f32),
        "norm_mix_g": gain(ks[5], (L, D)),
        "w_in": nrm(ks[6], (L, D, IN_WIDTH), D),
        "w_pool": nrm(ks[7], (L, POOL_GROUPS, POOL_GROUP_DIM, POOL_GROUP_DIM), POOL_GROUP_DIM),
        "pool_scale": gain(ks[8], (L, POOL_WIDTH)),
        "q_norm_g": gain(ks[9], (L, Q_LORA_RANK)),
        "w_uq": nrm(ks[10], (L, Q_LORA_RANK, MLA_HEADS * QK_HEAD_DIM), Q_LORA_RANK),
        "kv_norm_g": gain(ks[11], (L, KV_LORA_RANK)),
        "w_ukv": nrm(ks[12], (L, KV_LORA_RANK, MLA_HEADS * (QK_NOPE_DIM + V_HEAD_DIM)), KV_LORA_RANK),
        "w_o": nrm(ks[13], (L, MIX_WIDTH, D), MIX_WIDTH),
        "norm_ffn_g": gain(ks[14], (L, D)),
        "w_group": nrm(ks[15], (L, D, N_GROUPS), D),
        "b_group": 0.01 * jax.random.normal(ks[16], (L, N_GROUPS), f32),
        "w_router": nrm(ks[17], (L, D, N_EXPERTS), D),
        "b_router": 0.01 * jax.random.normal(ks[18], (L, N_EXPERTS), f32),
        "w_gate_up": nrm(ks[19], (L, N_EXPERTS, D, 2 * D_EXPERT), D),
        "w_down": nrm(ks[20], (L, N_EXPERTS, D_EXPERT, D), D_EXPERT),
        "final_g": gain(ks[21], (D,)),
    }


def reference(x, c, positions, w_mod, b_mod, norm_mix_g, w_in, w_pool, pool_scale,
              q_norm_g, w_uq, kv_norm_g, w_ukv, w_o, norm_ffn_g, w_group, b_group,
              w_router, b_router, w_gate_up, w_down, final_g):
    c_act = jax.nn.silu(c)
    cut1 = POOL_WIDTH
    cut2 = cut1 + Q_LORA_RANK
    cut3 = cut2 + KV_LORA_RANK
    for l in range(DEPTH):
        mod = (c_act @ w_mod[l] + b_mod[l])[:, None, :]
        shift_a, scale_a, gate_a, shift_f, scale_f, gate_f = jnp.split(mod, N_MOD, axis=-1)
        h = rmsnorm(x, norm_mix_g[l]) * (1.0 + scale_a) + shift_a
        u = h @ w_in[l]
        y_pool = pool_mixer(u[..., :cut1], w_pool[l], pool_scale[l])
        y_mla = mla_attention(u[..., cut1:cut2], u[..., cut2:cut3], u[..., cut3:], positions,
                              q_norm_g[l], w_uq[l], kv_norm_g[l], w_ukv[l])
        mix = jnp.concatenate([y_pool, y_mla], axis=-1) @ w_o[l]
        x = x + gate_a * mix
        h = rmsnorm(x, norm_ffn_g[l]) * (1.0 + scale_f) + shift_f
        x = x + gate_f * hierarchical_moe(h, w_group[l], b_group[l], w_router[l], b_router[l],
                                          w_gate_up[l], w_down[l])
    return rmsnorm(x, final_g)
```

```python
import contextlib
import math
import numpy as np
import ml_dtypes
import concourse.bass as bass
import concourse.mybir as mybir
from concourse.bass_utils import run_bass_kernel_spmd

F32 = mybir.dt.float32
BF16 = mybir.dt.bfloat16
I32 = mybir.dt.int32
AF = mybir.ActivationFunctionType
ALU = mybir.AluOpType
AX = mybir.AxisListType

S = 4096
D = 1024
NBLK = 8
EPS = 1e-6
NEXP = 32
TWO_PI = 2.0 * math.pi
SM_SCALE = 192 ** -0.5


class Buf:
    def __init__(self):
        self.w = {}
        self.r = {}
        self.dsem = None
        self.dcnt = 0
        self.dram = False


class V:
    def __init__(self, ap, buf):
        self.ap = ap
        self.buf = buf

    def __getitem__(self, idx):
        return V(self.ap[idx], self.buf)

    def rearrange(self, s, **kw):
        return V(self.ap.rearrange(s, **kw), self.buf)


class T:
    def __init__(self, h, buf=None):
        self.h = h
        self.buf = buf or Buf()

    def __getitem__(self, idx):
        return V(self.h[idx], self.buf)

    def bitcast(self, dt):
        return T(self.h.bitcast(dt), self.buf)


class Eng:
    def __init__(self, ctx, e, name, same_wait):
        self.e = e
        self.name = name
        self.sem = ctx.newsem("e_" + name)
        self.n = 0
        self.seen = {}
        self.same_wait = same_wait

    def wait_tok(self, sem, val):
        if val <= 0:
            return
        if sem is self.sem and not self.same_wait:
            return
        if self.seen.get(id(sem), 0) >= val:
            return
        self.e.wait_ge(sem, val)
        self.seen[id(sem)] = val


def _merge(d, src):
    for k, (s, v) in src.items():
        if k not in d or d[k][1] < v:
            d[k] = (s, v)


class Ctx:
    def __init__(self, nc, es):
        self.nc = nc
        self.es = es
        self.nsem = 0
        self.dma_sems = []
        self.PE = Eng(self, nc.tensor, "pe", False)
        self.ACT = Eng(self, nc.scalar, "act", True)
        self.DVE = Eng(self, nc.vector, "dve", True)
        self.POOL = Eng(self, nc.gpsimd, "pool", True)
        self.SP = Eng(self, nc.sync, "sp", False)
        self.engs = [self.PE, self.ACT, self.DVE, self.POOL, self.SP]

    def newsem(self, name):
        self.nsem += 1
        return self.es.enter_context(self.nc.semaphore(f"{name}_{self.nsem}"))

    def sb(self, stack, name, shape, dt):
        return T(stack.enter_context(self.nc.sbuf_tensor("sb_" + name, list(shape), dt)))

    def emit(self, E, name, inc=True, **kw):
        reads, writes, args = [], [], {}
        for k, v in kw.items():
            if isinstance(v, V):
                (writes if k in ("out", "accum_out", "ap") else reads).append(v.buf)
                args[k] = v.ap
            else:
                args[k] = v
        deps = {}
        for b in reads:
            _merge(deps, b.w)
        for b in writes:
            _merge(deps, b.w)
            _merge(deps, b.r)
        for s, v in deps.values():
            E.wait_tok(s, v)
        ins = getattr(E.e, name)(**args)
        if inc:
            E.n += 1
            ins.then_inc(E.sem, 1)
            val = E.n
        else:
            val = E.n + 1
        key = id(E.sem)
        for b in reads:
            if b.r.get(key, (None, 0))[1] < val:
                b.r[key] = (E.sem, val)
        for b in writes:
            b.w[key] = (E.sem, val)
        return ins

    def dma(self, Q, out, in_, **kw):
        reads, writes = [], []
        oap, iap = out, in_
        if isinstance(out, V):
            writes.append(out.buf)
            oap = out.ap
        if isinstance(in_, V):
            reads.append(in_.buf)
            iap = in_.ap
        deps = {}
        for b in reads:
            _merge(deps, b.w)
        for b in writes:
            _merge(deps, b.w)
            _merge(deps, b.r)
        for s, v in deps.values():
            Q.wait_tok(s, v)
        cand = [b for b in (writes + reads) if not b.dram]
        prim = (cand or (writes + reads))[0]
        if prim.dsem is None or prim.dcnt >= 480:
            prim.dsem = self.newsem("d")
            prim.dcnt = 0
            prim.rec = [prim.dsem, 0]
            self.dma_sems.append(prim.rec)
        ins = Q.e.dma_start(out=oap, in_=iap, **kw)
        prim.dcnt += 16
        prim.rec[1] = prim.dcnt
        ins.then_inc(prim.dsem, 16)
        key = id(prim.dsem)
        for b in reads:
            b.r[key] = (prim.dsem, prim.dcnt)
        for b in writes:
            b.w[key] = (prim.dsem, prim.dcnt)
        return ins

    def barrier(self):
        for E in self.engs:
            for X in self.engs:
                if X is not E:
                    E.wait_tok(X.sem, X.n)
            for sem_, cnt_ in self.dma_sems:
                E.wait_tok(sem_, cnt_)


def build(stage=2, nblk=NBLK, dbg=False, cut=9):
    nc = bass.Bass("TRN2", target_bir_lowering=False)

    def din(name, shape, dt=F32):
        return nc.dram_tensor(name, list(shape), dt, kind="ExternalInput").ap()

    x = din("x", [S, D])
    pos = din("pos", [1, S], I32)
    small = din("small", [80, 128])
    bmod_row = din("bmod_row", [1, 6144])
    final_g = din("final_g", [D])
    b_gr = din("b_gr", [36])
    w_mod = din("w_mod", [D, 6144])
    w_in = din("w_in", [D, 1088])
    w_pool = din("w_pool", [4, 64, 64])
    w_uq = din("w_uq", [512, 1152])
    w_ukv = din("w_ukv", [256, 1536])
    w_o = din("w_o", [D, D])
    w_gr = din("w_gr", [D, 36])
    w_gu = din("w_gate_up", [NEXP, D, 512])
    w_dn = din("w_down", [NEXP, 256, D])
    ident_bf = din("ident_bf", [128, 128], BF16)
    ident_f = din("ident_f", [128, 128])
    tri = din("tri", [128, 128], BF16)
    ropec = din("ropec", [64, 2])
    poolc = din("poolc", [128, 34])
    y = nc.dram_tensor("y", [S, D], F32, kind="ExternalOutput").ap()
    dbg_h = nc.dram_tensor("dbg", [128, 1024], F32, kind="ExternalOutput").ap() if dbg else None
    x1s_h = nc.dram_tensor("x1s", [S, D], F32, kind="Internal").ap()
    grow_h = nc.dram_tensor("grow_d", [1, 2048], F32, kind="Internal").ap()

    es = contextlib.ExitStack()
    with es:
        cx = Ctx(nc, es)
        PE, ACT, DVE, POOL, SP = cx.PE, cx.ACT, cx.DVE, cx.POOL, cx.SP
        emit, dma = cx.emit, cx.dma
        x1s = [V(x1s_h[t * 128:(t + 1) * 128, :], Buf()) for t in range(32)]
        yv = [V(y[t * 128:(t + 1) * 128, :], Buf()) for t in range(32)]
        grow_b = Buf()
        grow_b.dram = True
        for v_ in x1s + yv:
            v_.buf.dram = True
        grow_d = V(grow_h, grow_b)

        PS = [T(es.enter_context(nc.psum_tensor(f"ps{i}", [128, 512], F32))) for i in range(8)]
        rot = {"i": 0}
        MISC = [PS[0], PS[1], PS[2], PS[7]]

        def nextps():
            p = MISC[rot["i"] % 4]
            rot["i"] += 1
            return p

        IDB = cx.sb(es, "idb", [128, 128], BF16)
        IDF = cx.sb(es, "idf", [128, 128], F32)
        TRI = cx.sb(es, "tri", [128, 128], BF16)
        ONESB = cx.sb(es, "onesb", [128, 128], BF16)
        ROPEC = cx.sb(es, "ropec", [64, 2], F32)
        POOLC = cx.sb(es, "poolc", [128, 34], F32)
        COLS = cx.sb(es, "cols", [128, 80], F32)
        MODC = cx.sb(es, "modc", [128, 48], F32)
        AB = cx.sb(es, "ab", [128, 16], F32)

        dma(SP, IDB[:, :], ident_bf)
        dma(SP, IDF[:, :], ident_f)
        dma(SP, TRI[:, :], tri)
        dma(SP, ROPEC[:, :], ropec)
        dma(SP, POOLC[:, :], poolc)
        emit(POOL, "memset", ap=ONESB[:, :], constant=1.0)

        es1 = contextlib.ExitStack()
        with es1:
            WIN = cx.sb(es1, "win", [128, 8, 1216], BF16)
            WUQ = cx.sb(es1, "wuq", [128, 4, 1600], BF16)
            WUKV = cx.sb(es1, "wukv", [128, 2, 1536], BF16)
            WO = cx.sb(es1, "wo", [128, 8, 1024], BF16)
            WP = cx.sb(es1, "wp", [128, 2, 128], BF16)

            es0 = contextlib.ExitStack()
            with es0:
                SMALL = cx.sb(es0, "small", [80, 128], F32)
                WM = [cx.sb(es0, f"wm{i}", [128, 8, 1024], BF16) for i in range(2)]
                CACT = cx.sb(es0, "cact", [128, 8], BF16)
                GROW = cx.sb(es0, "grow", [1, 2048], F32)
                BROW = cx.sb(es0, "brow", [1, 2048], F32)
                WOS = cx.sb(es0, "wos", [128, 8, 1024], F32)
                GABC = cx.sb(es0, "gabc", [128, 1024], F32)

                dma(SP, SMALL[:, :], small)
                dma(SP, BROW[0:1, 0:1024], bmod_row[0:1, 2048:3072])
                dma(SP, BROW[0:1, 1024:2048], bmod_row[0:1, 5120:6144])
                dma(SP, WOS[:, :, :], w_o.rearrange("(k p) n -> p k n", p=128))
                for piece in range(2):
                    dma(POOL, WM[piece][:, :, :],
                        w_mod[:, piece * 1024:(piece + 1) * 1024].rearrange("(k p) n -> p k n", p=128))

                emit(PE, "transpose", out=PS[7][:, 0:80], in_=SMALL[:, :], identity=IDF[0:80, 0:80])
                emit(ACT, "activation", out=COLS[:, :], in_=PS[7][:, 0:80], func=AF.Copy)
                emit(ACT, "activation", out=CACT[:, :], in_=COLS[:, 48:56], func=AF.Silu)

                for piece in range(6):
                    wm = WM[piece % 2]
                    for jj in range(8):
                        j = piece * 8 + jj
                        for k in range(8):
                            emit(PE, "matmul", out=PS[6][:, j:j + 1], lhsT=wm[:, k, jj * 128:(jj + 1) * 128],
                                 rhs=CACT[:, k:k + 1], start=(k == 0), stop=(k == 7), inc=(k == 7))
                    if piece in (2, 5):
                        gi = 0 if piece == 2 else 1
                        for half in range(2):
                            pb = PS[2 + gi * 2 + half]
                            for k in range(8):
                                emit(PE, "matmul", out=pb[0:1, :], lhsT=CACT[:, k:k + 1],
                                     rhs=wm[:, k, half * 512:(half + 1) * 512], start=(k == 0), stop=(k == 7),
                                     inc=(k == 7))
                            c0 = gi * 1024 + half * 512
                            emit(DVE, "tensor_tensor", out=GROW[0:1, c0:c0 + 512], in0=pb[0:1, :],
                                 in1=BROW[0:1, c0:c0 + 512], op=ALU.add)
                    if piece + 2 < 6:
                        dma(POOL, wm[:, :, :],
                            w_mod[:, (piece + 2) * 1024:(piece + 3) * 1024].rearrange("(k p) n -> p k n", p=128))
                emit(DVE, "tensor_tensor", out=MODC[:, :], in0=PS[6][:, 0:48], in1=COLS[:, 0:48], op=ALU.add)
                emit(DVE, "scalar_tensor_tensor", out=AB[:, 0:8], in0=MODC[:, 8:16], scalar=1.0,
                     in1=COLS[:, 56:64], op0=ALU.add, op1=ALU.mult)
                emit(DVE, "scalar_tensor_tensor", out=AB[:, 8:16], in0=MODC[:, 32:40], scalar=1.0,
                     in1=COLS[:, 64:72], op0=ALU.add, op1=ALU.mult)
                dma(SP, grow_d, GROW[0:1, :])
                dma(SP, GABC[:, :], V(grow_h[0, 0:1024].partition_broadcast(128), grow_b))

                emit(POOL, "memset", ap=WIN[:, :, 1152:1216], constant=0.0)
                emit(POOL, "memset", ap=WUQ[:, :, 1536:1600], constant=0.0)
                dma(POOL, WIN[:, :, 0:1088], w_in.rearrange("(k p) n -> p k n", p=128))
                dma(POOL, WIN[:, :, 1088:1120], w_in[:, 1056:1088].rearrange("(k p) n -> p k n", p=128))
                dma(POOL, WIN[:, :, 1120:1152], w_in[:, 1024:1056].rearrange("(k p) n -> p k n", p=128))
                dma(POOL, WUQ[:, :, 0:1152], w_uq.rearrange("(k p) n -> p k n", p=128))
                for h in range(6):
                    b0 = h * 192 + 128
                    dma(POOL, WUQ[:, :, 1152 + h * 64:1152 + h * 64 + 32],
                        w_uq[:, b0 + 32:b0 + 64].rearrange("(k p) n -> p k n", p=128))
                    dma(POOL, WUQ[:, :, 1152 + h * 64 + 32:1152 + h * 64 + 64],
                        w_uq[:, b0:b0 + 32].rearrange("(k p) n -> p k n", p=128))
                dma(POOL, WUKV[:, :, :], w_ukv.rearrange("(k p) n -> p k n", p=128))
                emit(POOL, "memset", ap=WP[:, :, :], constant=0.0)
                for g in range(4):
                    p0 = (g % 2) * 64
                    dma(POOL, WP[p0:p0 + 64, g // 2, p0:p0 + 64], w_pool[g])
                for k in range(8):
                    emit(DVE, "tensor_tensor", out=WO[:, k, :], in0=WOS[:, k, :], in1=GABC[:, :], op=ALU.mult)
                if dbg:
                    dma(SP, dbg_h[:, 0:48], MODC[:, :])
                    dma(SP, dbg_h[:, 48:64], AB[:, :])
                    dma(SP, dbg_h[:, 64:144], COLS[:, :])
                    dma(SP, dbg_h[:, 256:768], GABC[:, 0:512])
                cx.barrier()

            KN = cx.sb(es1, "kn", [128, 6, S], BF16)
            VT = cx.sb(es1, "vt", [128, 32, 768], BF16)
            KR = cx.sb(es1, "kr", [128, S], BF16)
            XT = [cx.sb(es1, f"xt{i}", [128, 1024], F32) for i in range(2)]
            XNS = [cx.sb(es1, f"xn{i}", [128, 1024], BF16) for i in range(2)]
            SSS = [cx.sb(es1, f"ss{i}", [128, 4], F32) for i in range(2)]
            HT = cx.sb(es1, "ht", [128, 8, 512], BF16)
            CAT = HT
            CQT = cx.sb(es1, "cqt", [128, 4, 512], BF16)
            CKVT = cx.sb(es1, "ckvt", [128, 2, 512], BF16)
            SQ = [cx.sb(es1, f"sq{i}", [128, 512], BF16) for i in range(2)]
            POOLED = cx.sb(es1, "pooled", [128, 2, 512], BF16)
            HALO = cx.sb(es1, "halo", [128, 2, 16], F32)
            COS2 = cx.sb(es1, "cos2", [64, 512], F32)
            SIN2 = cx.sb(es1, "sin2", [64, 512], F32)
            QN = [cx.sb(es1, f"qn{i}", [128, 512], BF16) for i in range(2)]
            QR = [cx.sb(es1, f"qr{i}", [128, 512], BF16) for i in range(2)]
            PT = [cx.sb(es1, f"pt{i}", [128, 512], BF16) for i in range(3)]
            RDEN = cx.sb(es1, "rden", [128, 512], F32)
            POSI = XT[1].bitcast(I32)[0:64, 0:512]
            RKVC = cx.sb(es1, "rkvc", [128, 8], F32)
            PTB = [PS[7].bitcast(BF16), PS[2].bitcast(BF16)]

            emit(DVE, "memset", ap=HALO[:, :, :], constant=0.0)
            emit(POOL, "memset", ap=KR[64:128, :], constant=0.0)
            for q_ in QR:
                emit(POOL, "memset", ap=q_[64:128, :], constant=0.0)
            A1 = AB

            for j in range(nblk):
                for tt in range(4):
                    t = 4 * j + tt
                    xt = XT[t % 2]
                    XN, SS, P7B = XNS[tt % 2], SSS[tt % 2], PTB[tt % 2]
                    dma(SP, xt[:, :], x[t * 128:(t + 1) * 128, :])
                    emit(ACT, "activation", out=XN[:, :], in_=xt[:, :], func=AF.Square, accum_out=SS[:, 0:1])
                    emit(ACT, "activation", out=SS[:, 1:2], in_=SS[:, 0:1], func=AF.Sqrt, scale=1.0 / D, bias=EPS)
                    emit(DVE, "reciprocal", out=SS[:, 2:3], in_=SS[:, 1:2])
                    emit(ACT, "activation", out=XN[:, :], in_=xt[:, :], func=AF.Copy, scale=SS[:, 2:3])
                    for k in range(8):
                        emit(PE, "transpose", out=P7B[:, k * 128:(k + 1) * 128], in_=XN[:, k * 128:(k + 1) * 128],
                             identity=IDB[:, :], inc=(k == 7))
                    for k in range(8):
                        emit(DVE, "tensor_scalar", out=HT[:, k, tt * 128:(tt + 1) * 128],
                             in0=P7B[:, k * 128:(k + 1) * 128], scalar1=A1[:, k:k + 1], scalar2=MODC[:, k:k + 1],
                             op0=ALU.mult, op1=ALU.add)

                if cut <= 1:
                    continue
                def proj(c0, c1):
                    ps = nextps()
                    c1 = c0 + 128
                    m = 128
                    for k in range(8):
                        emit(PE, "matmul", out=ps[0:m, :], lhsT=WIN[:, k, c0:c1], rhs=HT[:, k, :],
                             start=(k == 0), stop=(k == 7), inc=(k == 7))
                    return ps

                SCR0, SCR1 = XT[0], XT[1]
                POSF = SCR0[0:64, 0:512]
                ANG = SCR0[0:64, 512:1024]
                dma(SP, POSI[:, :], pos[0, j * 512:(j + 1) * 512].partition_broadcast(64))
                emit(DVE, "tensor_copy", out=POSF, in_=POSI[:, :])
                emit(DVE, "tensor_scalar", out=ANG, in0=POSF, scalar1=ROPEC[:, 0:1], scalar2=None, op0=ALU.mult)
                emit(DVE, "tensor_scalar", out=POSF, in0=ANG, scalar1=1.0 / TWO_PI, scalar2=None, op0=ALU.mult)
                emit(DVE, "tensor_copy", out=POSI[:, :], in_=POSF)
                emit(DVE, "tensor_copy", out=POSF, in_=POSI[:, :])
                emit(DVE, "scalar_tensor_tensor", out=ANG, in0=POSF, scalar=-TWO_PI, in1=ANG, op0=ALU.mult,
                     op1=ALU.add)
                emit(DVE, "tensor_scalar", out=POSF, in0=ANG, scalar1=math.pi, scalar2=None, op0=ALU.is_gt)
                emit(DVE, "scalar_tensor_tensor", out=ANG, in0=POSF, scalar=-TWO_PI, in1=ANG, op0=ALU.mult,
                     op1=ALU.add)
                emit(DVE, "tensor_scalar", out=POSF, in0=ANG, scalar1=-math.pi, scalar2=None, op0=ALU.is_lt)
                emit(DVE, "scalar_tensor_tensor", out=ANG, in0=POSF, scalar=TWO_PI, in1=ANG, op0=ALU.mult,
                     op1=ALU.add)
                emit(DVE, "tensor_scalar", out=POSF, in0=ANG, scalar1=math.pi / 2, scalar2=None, op0=ALU.is_gt)
                emit(DVE, "scalar_tensor_tensor", out=POSF, in0=POSF, scalar=-TWO_PI, in1=ANG, op0=ALU.mult,
                     op1=ALU.add)
                emit(DVE, "tensor_scalar", out=POSF, in0=POSF, scalar1=math.pi / 2, scalar2=None, op0=ALU.add)
                emit(DVE, "tensor_scalar", out=POSF, in0=POSF, scalar1=-3.14159, scalar2=3.14159, op0=ALU.max,
                     op1=ALU.min)
                emit(DVE, "tensor_scalar", out=ANG, in0=ANG, scalar1=-3.14159, scalar2=3.14159, op0=ALU.max,
                     op1=ALU.min)
                emit(ACT, "activation", out=COS2[:, :], in_=POSF, func=AF.Sin)
                emit(ACT, "activation", out=SIN2[:, :], in_=ANG, func=AF.Sin)
                emit(DVE, "tensor_scalar", out=SIN2[:, :], in0=SIN2[:, :], scalar1=ROPEC[:, 1:2], scalar2=None,
                     op0=ALU.mult)

                for c in range(2):
                    ps = proj(c * 128, (c + 1) * 128)
                    for hh in range(2):
                        PB = SCR0[:, 0:272]
                        T1 = SCR0[:, 272:544]
                        T2 = SCR0[:, 544:816]
                        TM = SCR0[:, 816:832]
                        emit(POOL, "tensor_copy", out=PB[:, 0:16], in_=HALO[:, c, :])
                        emit(ACT, "activation", out=PB[:, 16:272], in_=ps[:, hh * 256:(hh + 1) * 256], func=AF.Copy)
                        emit(POOL, "tensor_copy", out=HALO[:, c, :], in_=PB[:, 256:272])
                        emit(POOL, "tensor_tensor", out=T1[:, 1:272], in0=PB[:, 1:272], in1=PB[:, 0:271], op=ALU.add)
                        if c == 0:
                            emit(POOL, "tensor_tensor", out=T2[64:128, 3:272], in0=T1[64:128, 3:272],
                                 in1=T1[64:128, 1:270], op=ALU.add)
                        else:
                            emit(POOL, "tensor_tensor", out=T2[:, 3:272], in0=T1[:, 3:272], in1=T1[:, 1:270],
                                 op=ALU.add)
                            emit(POOL, "tensor_tensor", out=T1[:, 7:272], in0=T2[:, 7:272], in1=T2[:, 3:268],
                                 op=ALU.add)
                            emit(POOL, "tensor_tensor", out=T2[64:128, 15:272], in0=T1[64:128, 15:272],
                                 in1=T1[64:128, 7:264], op=ALU.add)
                        for (p0, p1, src) in ((0, 64, T1), (64, 128, T2)):
                            fix = (j == 0 and hh == 0)
                            if fix:
                                emit(POOL, "tensor_tensor", out=TM[p0:p1, :], in0=src[p0:p1, 16:32],
                                     in1=POOLC[p0:p1, c * 17 + 1:c * 17 + 17], op=ALU.mult)
                                emit(POOL, "tensor_tensor", out=TM[p0:p1, :], in0=TM[p0:p1, :],
                                     in1=PB[p0:p1, 16:32], op=ALU.subtract)
                            emit(POOL, "tensor_scalar", out=src[p0:p1, 16:272], in0=src[p0:p1, 16:272],
                                 scalar1=POOLC[p0:p1, c * 17:c * 17 + 1], scalar2=None, op0=ALU.mult)
                            emit(POOL, "tensor_tensor", out=POOLED[p0:p1, c, hh * 256:(hh + 1) * 256],
                                 in0=src[p0:p1, 16:272], in1=PB[p0:p1, 16:272], op=ALU.subtract)
                            if fix:
                                emit(POOL, "tensor_copy", out=POOLED[p0:p1, c, 0:16], in_=TM[p0:p1, :])

                for i in range(4):
                    ps = proj(256 + i * 128, 256 + (i + 1) * 128)
                    emit(ACT, "activation", out=CQT[:, i, :], in_=ps[:, :], func=AF.Copy, scale=COLS[:, 74 + i:75 + i])
                    emit(ACT, "activation", out=SQ[i % 2][:, :], in_=ps[:, :], func=AF.Square)
                    emit(PE, "matmul", out=PS[3][:, :], lhsT=ONESB[:, :], rhs=SQ[i % 2][:, :], start=(i == 0),
                         stop=(i == 3))
                for i in range(2):
                    ps = proj(768 + i * 128, 768 + (i + 1) * 128)
                    emit(ACT, "activation", out=CKVT[:, i, :], in_=ps[:, :], func=AF.Copy, scale=COLS[:, 78 + i:79 + i])
                    emit(ACT, "activation", out=SQ[i][:, :], in_=ps[:, :], func=AF.Square)
                for i in range(2):
                    emit(PE, "matmul", out=PS[4][:, :], lhsT=ONESB[:, :], rhs=SQ[i][:, :], start=(i == 0), stop=(i == 1))
                for tt in range(4):
                    for i in range(2):
                        emit(PE, "matmul", out=PS[5][:, tt:tt + 1], lhsT=SQ[i][:, tt * 128:(tt + 1) * 128],
                             rhs=ONESB[:, 0:1], start=(i == 0), stop=(i == 1), inc=(i == 1))
                RQ = SCR1[:, 0:512]
                RKV = SCR1[:, 512:1024]
                emit(ACT, "activation", out=RQ, in_=PS[3][:, :], func=AF.Sqrt, scale=1.0 / 512, bias=EPS)
                emit(DVE, "reciprocal", out=RQ, in_=RQ)
                emit(DVE, "tensor_scalar", out=RQ, in0=RQ, scalar1=SM_SCALE, scalar2=None, op0=ALU.mult)
                emit(ACT, "activation", out=RKV, in_=PS[4][:, :], func=AF.Sqrt, scale=1.0 / 256, bias=EPS)
                emit(DVE, "reciprocal", out=RKV, in_=RKV)
                emit(ACT, "activation", out=RKVC[:, 0:4], in_=PS[5][:, 0:4], func=AF.Sqrt, scale=1.0 / 256, bias=EPS)
                emit(DVE, "reciprocal", out=RKVC[:, 4:8], in_=RKVC[:, 0:4])

                ps1 = proj(1024, 1088)
                ps2 = proj(1088, 1152)
                TA = SCR0[0:64, 0:512]
                TB = SCR0[0:64, 512:1024]
                emit(DVE, "tensor_tensor", out=TA, in0=ps1[0:64, :], in1=COS2[:, :], op=ALU.mult)
                emit(DVE, "tensor_tensor", out=TB, in0=ps2[0:64, :], in1=SIN2[:, :], op=ALU.mult)
                emit(DVE, "tensor_tensor", out=KR[0:64, j * 512:(j + 1) * 512], in0=TA, in1=TB, op=ALU.add)

                for h in range(6):
                    ps = nextps()
                    for k in range(2):
                        emit(PE, "matmul", out=ps[:, :], lhsT=WUKV[:, k, h * 256:h * 256 + 128], rhs=CKVT[:, k, :],
                             start=(k == 0), stop=(k == 1), inc=(k == 1))
                    emit(DVE, "tensor_tensor", out=KN[:, h, j * 512:(j + 1) * 512], in0=ps[:, :], in1=RKV,
                         op=ALU.mult)
                for tt in range(4):
                    t = 4 * j + tt
                    for half in range(2):
                        ps = nextps()
                        for k in range(2):
                            rhs = WUKV[:, k, :].rearrange("p (h t d) -> p h t d", t=2, d=128)[:, half * 3:half * 3 + 3, 1, :]
                            emit(PE, "matmul", out=ps[:, 0:384].rearrange("p (h d) -> p h d", d=128),
                                 lhsT=CKVT[:, k, tt * 128:(tt + 1) * 128], rhs=rhs, start=(k == 0), stop=(k == 1),
                                 inc=(k == 1))
                        emit(ACT, "activation", out=VT[:, t, half * 384:(half + 1) * 384], in_=ps[:, 0:384],
                             func=AF.Copy, scale=RKVC[:, 4 + tt:5 + tt])

                if cut <= 2:
                    continue
                def qproj(h):
                    ps = nextps()
                    for k in range(4):
                        emit(PE, "matmul", out=ps[:, :], lhsT=WUQ[:, k, h * 192:h * 192 + 128], rhs=CQT[:, k, :],
                             start=(k == 0), stop=(k == 3), inc=(k == 3))
                    emit(DVE, "tensor_tensor", out=QN[h % 2][:, :], in0=ps[:, :], in1=RQ, op=ALU.mult)
                    pa = nextps()
                    for k in range(4):
                        emit(PE, "matmul", out=pa[:, :], lhsT=WUQ[:, k, h * 192 + 128:h * 192 + 256],
                             rhs=CQT[:, k, :], start=(k == 0), stop=(k == 3), inc=(k == 3))
                    pb = nextps()
                    for k in range(4):
                        emit(PE, "matmul", out=pb[:, :], lhsT=WUQ[:, k, 1152 + h * 64:1152 + h * 64 + 128],
                             rhs=CQT[:, k, :], start=(k == 0), stop=(k == 3), inc=(k == 3))
                    emit(DVE, "tensor_tensor", out=TA, in0=pa[0:64, :], in1=COS2[:, :], op=ALU.mult)
                    emit(DVE, "tensor_tensor", out=TB, in0=pb[0:64, :], in1=SIN2[:, :], op=ALU.mult)
                    emit(DVE, "tensor_tensor", out=TA, in0=TA, in1=TB, op=ALU.add)
                    emit(DVE, "tensor_tensor", out=QR[h % 2][0:64, :], in0=TA, in1=SCR1[0:64, 0:512], op=ALU.mult)

                nk = 4 * j + 4
                pcnt = [0]

                def s_tile(h, i):
                    r = i - 4 * j
                    qo = 128 * r if r > 0 else 0
                    n = 512 - qo
                    ps = nextps()
                    emit(PE, "matmul", out=ps[:, 0:n], lhsT=KN[:, h, i * 128:(i + 1) * 128], rhs=QN[h % 2][:, qo:512],
                         start=True, stop=False, inc=False)
                    emit(PE, "matmul", out=ps[:, 0:n], lhsT=KR[:, i * 128:(i + 1) * 128], rhs=QR[h % 2][:, qo:512],
                         start=False, stop=True)
                    return ps, qo, n, r

                def attention(h, prev_final):
                    OB = PS[3] if h % 2 == 0 else PS[5]
                    DB = PS[4] if h % 2 == 0 else PS[6]
                    queue = [s_tile(h, i) for i in range(min(2, nk))]
                    for i in range(nk):
                        if i + 2 < nk:
                            queue.append(s_tile(h, i + 2))
                        ps, qo, n, r = queue.pop(0)
                        pt = PT[pcnt[0] % 3]
                        pcnt[0] += 1
                        emit(ACT, "activation", out=pt[:, 0:n], in_=ps[:, 0:n], func=AF.Exp)
                        if r >= 0:
                            emit(DVE, "tensor_tensor", out=pt[:, 0:128], in0=pt[:, 0:128], in1=TRI[:, :], op=ALU.mult)
                        emit(PE, "matmul", out=OB[:, qo:512], lhsT=VT[:, i, h * 128:(h + 1) * 128], rhs=pt[:, 0:n],
                             start=(i == 0), stop=(i == nk - 1))
                        if i == 0:
                            emit(DVE, "tensor_copy", out=DB[:, :], in_=pt[:, :])
                        else:
                            emit(DVE, "tensor_tensor", out=DB[:, qo:512], in0=DB[:, qo:512], in1=pt[:, 0:n], op=ALU.add)
                        if i == 1 and prev_final is not None:
                            prev_final()
                            prev_final = None
                    if prev_final is not None:
                        prev_final()

                    def final():
                        emit(DVE, "tensor_copy", out=SQ[0][:, :], in_=DB[:, :])
                        pd = nextps()
                        emit(PE, "matmul", out=pd[:, :], lhsT=ONESB[:, :], rhs=SQ[0][:, :], start=True, stop=True)
                        emit(DVE, "reciprocal", out=RDEN[:, :], in_=pd[:, :])
                        emit(DVE, "tensor_tensor", out=CAT[:, 2 + h, :], in0=OB[:, :], in1=RDEN[:, :], op=ALU.mult)
                    return final

                qproj(0)
                fin = None
                for h in range(6):
                    if h + 1 < 6:
                        qproj(h + 1)
                    fin = attention(h, fin)
                fin()

                for c in range(2):
                    ps = nextps()
                    emit(PE, "matmul", out=ps[:, :], lhsT=WP[:, c, :], rhs=POOLED[:, c, :], start=True, stop=True)
                    emit(ACT, "activation", out=CAT[:, c, :], in_=ps[:, :], func=AF.Copy, scale=COLS[:, 72 + c:73 + c])

                if cut <= 3:
                    continue
                for tt in range(4):
                    t = 4 * j + tt
                    xr = XT[tt % 2]
                    dma(SP, xr[:, :], x[t * 128:(t + 1) * 128, :])
                    for dh in range(2):
                        ps = nextps()
                        for fc in range(8):
                            emit(PE, "matmul", out=ps[:, :], lhsT=CAT[:, fc, tt * 128:(tt + 1) * 128],
                                 rhs=WO[:, fc, dh * 512:(dh + 1) * 512], start=(fc == 0), stop=(fc == 7),
                                 inc=(fc == 7))
                        emit(DVE, "tensor_tensor", out=xr[:, dh * 512:(dh + 1) * 512], in0=ps[:, :],
                             in1=xr[:, dh * 512:(dh + 1) * 512], op=ALU.add)
                    dma(SP, x1s[t], xr[:, :])
                    if stage == 1:
                        dma(SP, yv[t], xr[:, :])
            cx.barrier()

        if stage >= 2:
            es2 = contextlib.ExitStack()
            with es2:
                NS = 4
                WGU = [cx.sb(es2, f"wgu{i}", [128, 8, 512], BF16) for i in range(2)]
                WD = [cx.sb(es2, f"wd{i}", [128, 2, 1024], BF16) for i in range(2)]
                H2T = [cx.sb(es2, f"h2t{i}", [128, 8, 1024], BF16) for i in range(2)]
                ACC = [[cx.sb(es2, f"acc{i}_{d_}", [128, 8, 512], F32) for d_ in range(2)] for i in range(2)]
                YS = [cx.sb(es2, f"ys{i}", [128, 512], F32) for i in range(2)]
                WT = [cx.sb(es2, f"wt{i}", [128, 8, 32], F32) for i in range(2)]
                XT2 = [cx.sb(es2, f"xm{i}", [128, 1024], F32) for i in range(2)]
                XE = [cx.sb(es2, f"xe{i}", [128, 1024], F32) for i in range(2)]
                XNF = cx.sb(es2, "xnf", [128, 1024], F32)
                H2F = cx.sb(es2, "h2f", [128, 8, 128], F32)
                SG = [cx.sb(es2, f"sg{i}", [128, 512], F32) for i in range(2)]
                AT = [cx.sb(es2, f"at{i}", [128, 2, 512], BF16) for i in range(2)]
                GFBC = cx.sb(es2, "gfbc", [128, 1024], F32)
                FGBC = cx.sb(es2, "fgbc", [128, 1024], F32)
                WGR = cx.sb(es2, "wgr", [128, 8, 36], F32)
                BGR = cx.sb(es2, "bgr", [128, 36], F32)
                LG = cx.sb(es2, "lg", [128, 36], F32)
                RT = cx.sb(es2, "rt", [128, 128], F32)
                SS2 = cx.sb(es2, "ss2", [128, 4], F32)
                SS3 = cx.sb(es2, "ss3", [128, 4], F32)
                YT = [cx.sb(es2, f"yt{i}", [128, 1024], F32) for i in range(2)]
                JUNK = cx.sb(es2, "junk", [128, 1024], BF16)
                JUNK2 = cx.sb(es2, "junk2", [128, 1024], BF16)

                dma(SP, GFBC[:, :], V(grow_h[0, 1024:2048].partition_broadcast(128), grow_b))
                dma(SP, FGBC[:, :], final_g.partition_broadcast(128))
                dma(SP, WGR[:, :, :], w_gr.rearrange("(k p) n -> p k n", p=128))
                dma(SP, BGR[:, :], b_gr.partition_broadcast(128))
                A2 = AB[:, 8:16]
                PP = PS[7]

                def load_expert(ge):
                    e = ge % NEXP
                    dma(POOL, WGU[ge % 2][:, :, :], w_gu[e].rearrange("(k p) n -> p k n", p=128))
                    dma(POOL, WD[ge % 2][:, :, :], w_dn[e].rearrange("(k p) n -> p k n", p=128))

                pro_cnt = [0]

                def prologue_stages(sb_, tl):
                    t = sb_ * 8 + tl
                    h2t, wt = H2T[sb_ % 2], WT[sb_ % 2]
                    def st0():
                        xt = XT2[pro_cnt[0] % 2]
                        pro_cnt[0] += 1
                        dma(SP, xt[:, :], x1s[t])
                        emit(ACT, "activation", out=JUNK[:, :], in_=xt[:, :], func=AF.Square, accum_out=SS2[:, 0:1])
                        emit(ACT, "activation", out=SS2[:, 1:2], in_=SS2[:, 0:1], func=AF.Sqrt, scale=1.0 / D, bias=EPS)
                        emit(DVE, "reciprocal", out=SS2[:, 2:3], in_=SS2[:, 1:2])
                        emit(ACT, "activation", out=XNF[:, :], in_=xt[:, :], func=AF.Copy, scale=SS2[:, 2:3])
                    def st1():
                        for half in (0,):
                            for kk in range(4):
                                k = half * 4 + kk
                                emit(PE, "transpose", out=PP[:, kk * 128:(kk + 1) * 128],
                                     in_=XNF[:, k * 128:(k + 1) * 128], identity=IDF[:, :], inc=(kk == 3))
                            for kk in range(4):
                                k = half * 4 + kk
                                emit(DVE, "tensor_scalar", out=H2F[:, k, :], in0=PP[:, kk * 128:(kk + 1) * 128],
                                     scalar1=A2[:, k:k + 1], scalar2=MODC[:, 24 + k:25 + k], op0=ALU.mult,
                                     op1=ALU.add)
                    def st2():
                        for half in (1,):
                            for kk in range(4):
                                k = half * 4 + kk
                                emit(PE, "transpose", out=PP[:, kk * 128:(kk + 1) * 128],
                                     in_=XNF[:, k * 128:(k + 1) * 128], identity=IDF[:, :], inc=(kk == 3))
                            for kk in range(4):
                                k = half * 4 + kk
                                emit(DVE, "tensor_scalar", out=H2F[:, k, :], in0=PP[:, kk * 128:(kk + 1) * 128],
                                     scalar1=A2[:, k:k + 1], scalar2=MODC[:, 24 + k:25 + k], op0=ALU.mult,
                                     op1=ALU.add)
                        emit(ACT, "activation", out=h2t[:, :, tl * 128:(tl + 1) * 128], in_=H2F[:, :, :], func=AF.Copy)
                    def st3():
                        for k in range(8):
                            emit(PE, "matmul", out=PP[:, 0:36], lhsT=H2F[:, k, :], rhs=WGR[:, k, :], start=(k == 0),
                                 stop=(k == 7), inc=(k == 7))
                        emit(DVE, "tensor_copy", out=LG[:, :], in_=PP[:, 0:36])
                        GLB = RT[:, 0:4]
                        GM = RT[:, 4:5]
                        GOH = RT[:, 8:12]
                        GEX = RT[:, 12:16]
                        GS = RT[:, 16:17]
                        GP = RT[:, 17:18]
                        GM2 = RT[:, 18:19]
                        EIN = RT[:, 24:32]
                        ZB = RT[:, 32:40]
                        M1 = RT[:, 40:41]
                        OH1 = RT[:, 48:56]
                        OH2 = RT[:, 56:64]
                        PEX = RT[:, 64:72]
                        DEN = RT[:, 72:73]
                        WL = RT[:, 80:88]
                        emit(DVE, "tensor_tensor", out=GLB, in0=LG[:, 0:4], in1=BGR[:, 0:4], op=ALU.add)
                        emit(DVE, "tensor_reduce", out=GM, in_=GLB, axis=AX.X, op=ALU.max)
                        emit(DVE, "tensor_scalar", out=GOH, in0=GLB, scalar1=GM, scalar2=None, op0=ALU.is_ge)
                        emit(DVE, "tensor_reduce", out=GM2, in_=LG[:, 0:4], axis=AX.X, op=ALU.max)
                        emit(DVE, "tensor_scalar", out=GEX, in0=LG[:, 0:4], scalar1=GM2, scalar2=None, op0=ALU.subtract)
                        emit(ACT, "activation", out=GEX, in_=GEX, func=AF.Exp)
                        emit(DVE, "tensor_reduce", out=GS, in_=GEX, axis=AX.X, op=ALU.add)
                        emit(DVE, "tensor_tensor", out=GEX, in0=GEX, in1=GOH, op=ALU.mult)
                        emit(DVE, "tensor_reduce", out=GP, in_=GEX, axis=AX.X, op=ALU.add)
                        emit(DVE, "reciprocal", out=GS, in_=GS)
                        emit(DVE, "tensor_tensor", out=GP, in0=GP, in1=GS, op=ALU.mult)
                        emit(DVE, "tensor_scalar", out=EIN, in0=LG[:, 4:12], scalar1=RT[:, 8:9], scalar2=None, op0=ALU.mult)
                        emit(DVE, "tensor_scalar", out=ZB, in0=BGR[:, 4:12], scalar1=RT[:, 8:9], scalar2=None, op0=ALU.mult)
                        for g in range(1, 4):
                            emit(DVE, "scalar_tensor_tensor", out=EIN, in0=LG[:, 4 + 8 * g:12 + 8 * g],
                                 scalar=RT[:, 8 + g:9 + g], in1=EIN, op0=ALU.mult, op1=ALU.add)
                            emit(DVE, "scalar_tensor_tensor", out=ZB, in0=BGR[:, 4 + 8 * g:12 + 8 * g],
                                 scalar=RT[:, 8 + g:9 + g], in1=ZB, op0=ALU.mult, op1=ALU.add)
                        emit(DVE, "tensor_tensor", out=ZB, in0=ZB, in1=EIN, op=ALU.add)
                        emit(DVE, "tensor_reduce", out=M1, in_=ZB, axis=AX.X, op=ALU.max)
                        emit(DVE, "tensor_scalar", out=OH1, in0=ZB, scalar1=M1, scalar2=None, op0=ALU.is_ge)
                        emit(DVE, "scalar_tensor_tensor", out=ZB, in0=OH1, scalar=-1e30, in1=ZB, op0=ALU.mult, op1=ALU.add)
                        emit(DVE, "tensor_reduce", out=M1, in_=ZB, axis=AX.X, op=ALU.max)
                        emit(DVE, "tensor_scalar", out=OH2, in0=ZB, scalar1=M1, scalar2=None, op0=ALU.is_ge)
                        emit(DVE, "tensor_tensor", out=OH1, in0=OH1, in1=OH2, op=ALU.add)
                        emit(DVE, "tensor_reduce", out=M1, in_=EIN, axis=AX.X, op=ALU.max)
                        emit(DVE, "tensor_scalar", out=PEX, in0=EIN, scalar1=M1, scalar2=None, op0=ALU.subtract)
                        emit(ACT, "activation", out=PEX, in_=PEX, func=AF.Exp)
                        emit(DVE, "tensor_tensor", out=PEX, in0=PEX, in1=OH1, op=ALU.mult)
                        emit(DVE, "tensor_reduce", out=DEN, in_=PEX, axis=AX.X, op=ALU.add)
                        emit(DVE, "reciprocal", out=DEN, in_=DEN)
                        emit(DVE, "tensor_tensor", out=DEN, in0=DEN, in1=GP, op=ALU.mult)
                        emit(DVE, "tensor_scalar", out=WL, in0=PEX, scalar1=DEN, scalar2=None, op0=ALU.mult)
                        for g in range(4):
                            emit(DVE, "tensor_scalar", out=wt[:, tl, g * 8:(g + 1) * 8], in0=WL,
                                 scalar1=RT[:, 8 + g:9 + g], scalar2=None, op0=ALU.mult)

                    return [st0, st1, st2, st3]

                def prologue_tile(sb_, tl):
                    for f_ in prologue_stages(sb_, tl):
                        f_()

                epi_cnt = [0]

                def epilogue_stages(sb_, tl):
                    t = sb_ * 8 + tl
                    acc = ACC[sb_ % 2]
                    xt = XE[epi_cnt[0] % 2]
                    yt = YT[epi_cnt[0] % 2]
                    epi_cnt[0] += 1

                    def ea():
                        dma(SP, xt[:, :], x1s[t])
                        for d_ in range(2):
                            emit(POOL, "tensor_tensor", out=acc[d_][:, tl, :], in0=acc[d_][:, tl, :],
                                 in1=GFBC[:, d_ * 512:(d_ + 1) * 512], op=ALU.mult)
                            emit(POOL, "tensor_tensor", out=xt[:, d_ * 512:(d_ + 1) * 512],
                                 in0=xt[:, d_ * 512:(d_ + 1) * 512], in1=acc[d_][:, tl, :], op=ALU.add)

                    def eb():
                        emit(ACT, "activation", out=JUNK2[:, :], in_=xt[:, :], func=AF.Square, accum_out=SS3[:, 0:1])
                        emit(ACT, "activation", out=SS3[:, 1:2], in_=SS3[:, 0:1], func=AF.Sqrt, scale=1.0 / D, bias=EPS)
                        emit(DVE, "reciprocal", out=SS3[:, 2:3], in_=SS3[:, 1:2])
                        emit(DVE, "scalar_tensor_tensor", out=yt[:, :], in0=xt[:, :], scalar=SS3[:, 2:3],
                             in1=FGBC[:, :], op0=ALU.mult, op1=ALU.mult)
                        dma(SP, yv[t], yt[:, :])
                    return ea, eb

                def epilogue_tile(sb_, tl):
                    ea, eb = epilogue_stages(sb_, tl)
                    ea()
                    eb()

                dcnt = [0]
                ycnt = [0]

                def gu_unit(sb_, e, sb):
                    ge = sb_ * NEXP + e
                    wgu = WGU[ge % 2]
                    h2t = H2T[sb_ % 2]
                    at = AT[(ge * 2 + sb) % 2]
                    for fc in range(2):
                        pg = PS[fc * 2]
                        pu = PS[fc * 2 + 1]
                        for k in range(8):
                            emit(PE, "matmul", out=pg[:, :], lhsT=wgu[:, k, fc * 128:(fc + 1) * 128],
                                 rhs=h2t[:, k, sb * 512:(sb + 1) * 512], start=(k == 0), stop=(k == 7),
                                 inc=(k == 7))
                        for k in range(8):
                            emit(PE, "matmul", out=pu[:, :], lhsT=wgu[:, k, 256 + fc * 128:256 + (fc + 1) * 128],
                                 rhs=h2t[:, k, sb * 512:(sb + 1) * 512], start=(k == 0), stop=(k == 7),
                                 inc=(k == 7))
                        sg = SG[fc]
                        emit(ACT, "activation", out=sg[:, :], in_=pg[:, :], func=AF.Silu)
                        emit(DVE, "tensor_tensor", out=at[:, fc, :], in0=pu[:, :], in1=sg[:, :], op=ALU.mult)

                def dn_unit(sb_, e, sb):
                    ge = sb_ * NEXP + e
                    wd = WD[ge % 2]
                    acc, wt = ACC[sb_ % 2], WT[sb_ % 2]
                    at = AT[(ge * 2 + sb) % 2]
                    for tt in range(4):
                        tl = sb * 4 + tt
                        for dh in range(2):
                            po = PS[4 + dcnt[0] % 3]
                            dcnt[0] += 1
                            for fc in range(2):
                                emit(PE, "matmul", out=po[:, :], lhsT=at[:, fc, tt * 128:(tt + 1) * 128],
                                     rhs=wd[:, fc, dh * 512:(dh + 1) * 512], start=(fc == 0), stop=(fc == 1),
                                     inc=(fc == 1))
                            accv = acc[dh][:, tl, :]
                            if e == 0:
                                emit(DVE, "tensor_scalar", out=accv, in0=po[:, :], scalar1=wt[:, tl, e:e + 1],
                                     scalar2=None, op0=ALU.mult)
                            else:
                                emit(DVE, "scalar_tensor_tensor", out=accv, in0=po[:, :],
                                     scalar=wt[:, tl, e:e + 1], in1=accv, op0=ALU.mult, op1=ALU.add)

                load_expert(0)
                load_expert(1)
                for tl in range(8):
                    prologue_tile(0, tl)
                units = [(sb_, e, sb) for sb_ in range(NS) for e in range(NEXP) for sb in range(2)]
                gu_unit(*units[0])
                stage_q = []
                for ui, (sb_, e, sb) in enumerate(units):
                    if ui + 1 < len(units):
                        gu_unit(*units[ui + 1])
                    dn_unit(sb_, e, sb)
                    uu = e * 2 + sb
                    if uu == 0 and sb_ + 1 < NS:
                        stage_q = [f_ for k in range(8) for f_ in prologue_stages(sb_ + 1, k)]
                    if uu % 2 == 0 and uu // 2 < len(stage_q):
                        stage_q[uu // 2]()
                    if sb == 1:
                        ge = sb_ * NEXP + e
                        if ge + 2 < NS * NEXP:
                            load_expert(ge + 2)
                        if e % 4 == 1 and sb_ >= 1:
                            epi_pending = epilogue_stages(sb_ - 1, e // 4)
                            epi_pending[0]()
                        if e % 4 == 2 and sb_ >= 1:
                            epi_pending[1]()
                    if uu == 63:
                        stage_q = []
                for tl in range(8):
                    epilogue_tile(NS - 1, tl)

        for v in yv:
            for s, val in v.buf.w.values():
                SP.wait_tok(s, val)
        cx.barrier()
        if dbg:
            print("SEMCOUNTS", {E.name: E.n for E in cx.engs}, "dma max", max(c_ for _, c_ in cx.dma_sems), "nsem", cx.nsem)
    return nc


_CACHE = {}


def _consts():
    ident = np.eye(128, dtype=np.float32)
    tri = (np.arange(128)[None, :] >= np.arange(128)[:, None]).astype(np.float32)
    inv_freq = (10000.0 ** (-(np.arange(0, 64, 2, dtype=np.float32) / np.float32(64)))).astype(np.float32)
    ropec = np.zeros((64, 2), np.float32)
    ropec[:, 0] = np.concatenate([inv_freq, inv_freq])
    ropec[:32, 1] = -1.0
    ropec[32:, 1] = 1.0
    poolc = np.zeros((128, 2, 17), np.float32)
    wins = (2, 4, 8, 16)
    for c in range(2):
        for half in range(2):
            w = wins[c * 2 + half]
            sl = slice(half * 64, half * 64 + 64)
            poolc[sl, c, 0] = 1.0 / w
            poolc[sl, c, 1:17] = 1.0 / np.minimum(np.arange(1, 17), w)
    return dict(ident_bf=ident.astype(ml_dtypes.bfloat16), ident_f=ident, tri=tri.astype(ml_dtypes.bfloat16),
                ropec=ropec, poolc=poolc.reshape(128, 34))


def make_in_maps(inputs, cores):
    cst = _consts()
    f = lambda a: np.ascontiguousarray(a, dtype=np.float32)
    shared = dict(
        bmod_row=f(inputs["b_mod"][0]).reshape(1, 6144), final_g=f(inputs["final_g"]),
        b_gr=f(np.concatenate([inputs["b_group"][0], inputs["b_router"][0]])),
        w_mod=f(inputs["w_mod"][0]), w_in=f(inputs["w_in"][0]), w_pool=f(inputs["w_pool"][0]),
        w_uq=f(inputs["w_uq"][0]), w_ukv=f(inputs["w_ukv"][0]), w_o=f(inputs["w_o"][0]),
        w_gr=f(np.concatenate([inputs["w_group"][0], inputs["w_router"][0]], axis=1)),
        w_gate_up=f(inputs["w_gate_up"][0]), w_down=f(inputs["w_down"][0]), **cst)
    maps = []
    for b in cores:
        small = np.concatenate([
            f(inputs["b_mod"][0]).reshape(48, 128), f(inputs["c"][b]).reshape(8, 128),
            f(inputs["norm_mix_g"][0]).reshape(8, 128), f(inputs["norm_ffn_g"][0]).reshape(8, 128),
            f(inputs["pool_scale"][0]).reshape(2, 128), f(inputs["q_norm_g"][0]).reshape(4, 128),
            f(inputs["kv_norm_g"][0]).reshape(2, 128)], axis=0)
        m = dict(shared)
        m["x"] = f(inputs["x"][b])
        m["pos"] = np.ascontiguousarray(inputs["positions"][b], dtype=np.int32).reshape(1, S)
        m["small"] = np.ascontiguousarray(small)
        maps.append(m)
    return maps


def kernel(**inputs):
    inputs = {k: np.asarray(v) for k, v in inputs.items()}
    if "nc" not in _CACHE:
        _CACHE["nc"] = build(stage=2)
    nc = _CACHE["nc"]
    maps = make_in_maps(inputs, list(range(8)))
    res = run_bass_kernel_spmd(nc, maps, core_ids=list(range(8)))
    out = np.stack([np.asarray(r["y"], dtype=np.float32) for r in res.results], axis=0)
    return out
```

```python
import contextlib
import math
import numpy as np
import ml_dtypes
import concourse.bass as bass
import concourse.mybir as mybir
from concourse.bass_utils import run_bass_kernel_spmd

F32 = mybir.dt.float32
BF16 = mybir.dt.bfloat16
I32 = mybir.dt.int32
AF = mybir.ActivationFunctionType
ALU = mybir.AluOpType
AX = mybir.AxisListType

S = 4096
D = 1024
NBLK = 8
EPS = 1e-6
NEXP = 32
TWO_PI = 2.0 * math.pi
SM_SCALE = 192 ** -0.5


class Buf:
    def __init__(self):
        self.w = {}
        self.r = {}
        self.dsem = None
        self.dcnt = 0
        self.dram = False


class V:
    def __init__(self, ap, buf):
        self.ap = ap
        self.buf = buf

    def __getitem__(self, idx):
        return V(self.ap[idx], self.buf)

    def rearrange(self, s, **kw):
        return V(self.ap.rearrange(s, **kw), self.buf)


class T:
    def __init__(self, h, buf=None):
        self.h = h
        self.buf = buf or Buf()

    def __getitem__(self, idx):
        return V(self.h[idx], self.buf)

    def bitcast(self, dt):
        return T(self.h.bitcast(dt), self.buf)


class Eng:
    def __init__(self, ctx, e, name, same_wait):
        self.e = e
        self.name = name
        self.sem = ctx.newsem("e_" + name)
        self.n = 0
        self.seen = {}
        self.same_wait = same_wait

    def wait_tok(self, sem, val):
        if val <= 0:
            return
        if sem is self.sem and not self.same_wait:
            return
        if self.seen.get(id(sem), 0) >= val:
            return
        self.e.wait_ge(sem, val)
        self.seen[id(sem)] = val


def _merge(d, src):
    for k, (s, v) in src.items():
        if k not in d or d[k][1] < v:
            d[k] = (s, v)


class Ctx:
    def __init__(self, nc, es):
        self.nc = nc
        self.es = es
        self.nsem = 0
        self.dma_sems = []
        self.PE = Eng(self, nc.tensor, "pe", False)
        self.ACT = Eng(self, nc.scalar, "act", True)
        self.DVE = Eng(self, nc.vector, "dve", True)
        self.POOL = Eng(self, nc.gpsimd, "pool", True)
        self.SP = Eng(self, nc.sync, "sp", False)
        self.engs = [self.PE, self.ACT, self.DVE, self.POOL, self.SP]

    def newsem(self, name):
        self.nsem += 1
        return self.es.enter_context(self.nc.semaphore(f"{name}_{self.nsem}"))

    def sb(self, stack, name, shape, dt):
        return T(stack.enter_context(self.nc.sbuf_tensor("sb_" + name, list(shape), dt)))

    def emit(self, E, name, inc=True, **kw):
        reads, writes, args = [], [], {}
        for k, v in kw.items():
            if isinstance(v, V):
                (writes if k in ("out", "accum_out", "ap") else reads).append(v.buf)
                args[k] = v.ap
            else:
                args[k] = v
        deps = {}
        for b in reads:
            _merge(deps, b.w)
        for b in writes:
            _merge(deps, b.w)
            _merge(deps, b.r)
        for s, v in deps.values():
            E.wait_tok(s, v)
        ins = getattr(E.e, name)(**args)
        if inc:
            E.n += 1
            ins.then_inc(E.sem, 1)
            val = E.n
        else:
            val = E.n + 1
        key = id(E.sem)
        for b in reads:
            if b.r.get(key, (None, 0))[1] < val:
                b.r[key] = (E.sem, val)
        for b in writes:
            b.w[key] = (E.sem, val)
        return ins

    def dma(self, Q, out, in_, **kw):
        reads, writes = [], []
        oap, iap = out, in_
        if isinstance(out, V):
            writes.append(out.buf)
            oap = out.ap
        if isinstance(in_, V):
            reads.append(in_.buf)
            iap = in_.ap
        deps = {}
        for b in reads:
            _merge(deps, b.w)
        for b in writes:
            _merge(deps, b.w)
            _merge(deps, b.r)
        for s, v in deps.values():
            Q.wait_tok(s, v)
        cand = [b for b in (writes + reads) if not b.dram]
        prim = (cand or (writes + reads))[0]
        if prim.dsem is None or prim.dcnt >= 480:
            prim.dsem = self.newsem("d")
            prim.dcnt = 0
            prim.rec = [prim.dsem, 0]
            self.dma_sems.append(prim.rec)
        ins = Q.e.dma_start(out=oap, in_=iap, **kw)
        prim.dcnt += 16
        prim.rec[1] = prim.dcnt
        ins.then_inc(prim.dsem, 16)
        key = id(prim.dsem)
        for b in reads:
            b.r[key] = (prim.dsem, prim.dcnt)
        for b in writes:
            b.w[key] = (prim.dsem, prim.dcnt)
        return ins

    def barrier(self):
        for E in self.engs:
            for X in self.engs:
                if X is not E:
                    E.wait_tok(X.sem, X.n)
            for sem_, cnt_ in self.dma_sems:
                E.wait_tok(sem_, cnt_)


def build(stage=2, nblk=NBLK, dbg=False, cut=9):
    nc = bass.Bass("TRN2", target_bir_lowering=False)

    def din(name, shape, dt=F32):
        return nc.dram_tensor(name, list(shape), dt, kind="ExternalInput").ap()

    x = din("x", [S, D])
    pos = din("pos", [1, S], I32)
    small = din("small", [80, 128])
    bmod_row = din("bmod_row", [1, 6144])
    final_g = din("final_g", [D])
    b_gr = din("b_gr", [36])
    w_mod = din("w_mod", [D, 6144])
    w_in = din("w_in", [D, 1088])
    w_pool = din("w_pool", [4, 64, 64])
    w_uq = din("w_uq", [512, 1152])
    w_ukv = din("w_ukv", [256, 1536])
    w_o = din("w_o", [D, D])
    w_gr = din("w_gr", [D, 36])
    w_gu = din("w_gate_up", [NEXP, D, 512])
    w_dn = din("w_down", [NEXP, 256, D])
    ident_bf = din("ident_bf", [128, 128], BF16)
    ident_f = din("ident_f", [128, 128])
    tri = din("tri", [128, 128], BF16)
    ropec = din("ropec", [64, 2])
    poolc = din("poolc", [128, 34])
    y = nc.dram_tensor("y", [S, D], F32, kind="ExternalOutput").ap()
    dbg_h = nc.dram_tensor("dbg", [128, 1024], F32, kind="ExternalOutput").ap() if dbg else None
    x1s_h = nc.dram_tensor("x1s", [S, D], F32, kind="Internal").ap()
    grow_h = nc.dram_tensor("grow_d", [1, 2048], F32, kind="Internal").ap()

    es = contextlib.ExitStack()
    with es:
        cx = Ctx(nc, es)
        PE, ACT, DVE, POOL, SP = cx.PE, cx.ACT, cx.DVE, cx.POOL, cx.SP
        emit, dma = cx.emit, cx.dma
        x1s = [V(x1s_h[t * 128:(t + 1) * 128, :], Buf()) for t in range(32)]
        yv = [V(y[t * 128:(t + 1) * 128, :], Buf()) for t in range(32)]
        grow_b = Buf()
        grow_b.dram = True
        for v_ in x1s + yv:
            v_.buf.dram = True
        grow_d = V(grow_h, grow_b)

        PS = [T(es.enter_context(nc.psum_tensor(f"ps{i}", [128, 512], F32))) for i in range(8)]
        rot = {"i": 0}
        MISC = [PS[0], PS[1], PS[2], PS[7]]

        def nextps():
            p = MISC[rot["i"] % 4]
            rot["i"] += 1
            return p

        IDB = cx.sb(es, "idb", [128, 128], BF16)
        IDF = cx.sb(es, "idf", [128, 128], F32)
        TRI = cx.sb(es, "tri", [128, 128], BF16)
        ONESB = cx.sb(es, "onesb", [128, 128], BF16)
        ROPEC = cx.sb(es, "ropec", [64, 2], F32)
        POOLC = cx.sb(es, "poolc", [128, 34], F32)
        COLS = cx.sb(es, "cols", [128, 80], F32)
        MODC = cx.sb(es, "modc", [128, 48], F32)
        AB = cx.sb(es, "ab", [128, 16], F32)

        dma(SP, IDB[:, :], ident_bf)
        dma(SP, IDF[:, :], ident_f)
        dma(SP, TRI[:, :], tri)
        dma(SP, ROPEC[:, :], ropec)
        dma(SP, POOLC[:, :], poolc)
        emit(POOL, "memset", ap=ONESB[:, :], constant=1.0)

        es1 = contextlib.ExitStack()
        with es1:
            WIN = cx.sb(es1, "win", [128, 8, 1216], BF16)
            WUQ = cx.sb(es1, "wuq", [128, 4, 1600], BF16)
            WUKV = cx.sb(es1, "wukv", [128, 2, 1536], BF16)
            WO = cx.sb(es1, "wo", [128, 8, 1024], BF16)
            WP = cx.sb(es1, "wp", [128, 2, 128], BF16)

            es0 = contextlib.ExitStack()
            with es0:
                SMALL = cx.sb(es0, "small", [80, 128], F32)
                WM = [cx.sb(es0, f"wm{i}", [128, 8, 1024], BF16) for i in range(2)]
                CACT = cx.sb(es0, "cact", [128, 8], BF16)
                GROW = cx.sb(es0, "grow", [1, 2048], F32)
                BROW = cx.sb(es0, "brow", [1, 2048], F32)
                WOS = cx.sb(es0, "wos", [128, 8, 1024], F32)
                GABC = cx.sb(es0, "gabc", [128, 1024], F32)

                dma(SP, SMALL[:, :], small)
                dma(SP, BROW[0:1, 0:1024], bmod_row[0:1, 2048:3072])
                dma(SP, BROW[0:1, 1024:2048], bmod_row[0:1, 5120:6144])
                dma(SP, WOS[:, :, :], w_o.rearrange("(k p) n -> p k n", p=128))
                for piece in range(2):
                    dma(POOL, WM[piece][:, :, :],
                        w_mod[:, piece * 1024:(piece + 1) * 1024].rearrange("(k p) n -> p k n", p=128))

                emit(PE, "transpose", out=PS[7][:, 0:80], in_=SMALL[:, :], identity=IDF[0:80, 0:80])
                emit(ACT, "activation", out=COLS[:, :], in_=PS[7][:, 0:80], func=AF.Copy)
                emit(ACT, "activation", out=CACT[:, :], in_=COLS[:, 48:56], func=AF.Silu)

                for piece in range(6):
                    wm = WM[piece % 2]
                    for jj in range(8):
                        j = piece * 8 + jj
                        for k in range(8):
                            emit(PE, "matmul", out=PS[6][:, j:j + 1], lhsT=wm[:, k, jj * 128:(jj + 1) * 128],
                                 rhs=CACT[:, k:k + 1], start=(k == 0), stop=(k == 7), inc=(k == 7))
                    if piece in (2, 5):
                        gi = 0 if piece == 2 else 1
                        for half in range(2):
                            pb = PS[2 + gi * 2 + half]
                            for k in range(8):
                                emit(PE, "matmul", out=pb[0:1, :], lhsT=CACT[:, k:k + 1],
                                     rhs=wm[:, k, half * 512:(half + 1) * 512], start=(k == 0), stop=(k == 7),
                                     inc=(k == 7))
                            c0 = gi * 1024 + half * 512
                            emit(DVE, "tensor_tensor", out=GROW[0:1, c0:c0 + 512], in0=pb[0:1, :],
                                 in1=BROW[0:1, c0:c0 + 512], op=ALU.add)
                    if piece + 2 < 6:
                        dma(POOL, wm[:, :, :],
                            w_mod[:, (piece + 2) * 1024:(piece + 3) * 1024].rearrange("(k p) n -> p k n", p=128))
                emit(DVE, "tensor_tensor", out=MODC[:, :], in0=PS[6][:, 0:48], in1=COLS[:, 0:48], op=ALU.add)
                emit(DVE, "scalar_tensor_tensor", out=AB[:, 0:8], in0=MODC[:, 8:16], scalar=1.0,
                     in1=COLS[:, 56:64], op0=ALU.add, op1=ALU.mult)
                emit(DVE, "scalar_tensor_tensor", out=AB[:, 8:16], in0=MODC[:, 32:40], scalar=1.0,
                     in1=COLS[:, 64:72], op0=ALU.add, op1=ALU.mult)
                dma(SP, grow_d, GROW[0:1, :])
                dma(SP, GABC[:, :], V(grow_h[0, 0:1024].partition_broadcast(128), grow_b))

                emit(POOL, "memset", ap=WIN[:, :, 1152:1216], constant=0.0)
                emit(POOL, "memset", ap=WUQ[:, :, 1536:1600], constant=0.0)
                dma(POOL, WIN[:, :, 0:1088], w_in.rearrange("(k p) n -> p k n", p=128))
                dma(POOL, WIN[:, :, 1088:1120], w_in[:, 1056:1088].rearrange("(k p) n -> p k n", p=128))
                dma(POOL, WIN[:, :, 1120:1152], w_in[:, 1024:1056].rearrange("(k p) n -> p k n", p=128))
                dma(POOL, WUQ[:, :, 0:1152], w_uq.rearrange("(k p) n -> p k n", p=128))
                for h in range(6):
                    b0 = h * 192 + 128
                    dma(POOL, WUQ[:, :, 1152 + h * 64:1152 + h * 64 + 32],
                        w_uq[:, b0 + 32:b0 + 64].rearrange("(k p) n -> p k n", p=128))
                    dma(POOL, WUQ[:, :, 1152 + h * 64 + 32:1152 + h * 64 + 64],
                        w_uq[:, b0:b0 + 32].rearrange("(k p) n -> p k n", p=128))
                dma(POOL, WUKV[:, :, :], w_ukv.rearrange("(k p) n -> p k n", p=128))
                emit(POOL, "memset", ap=WP[:, :, :], constant=0.0)
                for g in range(4):
                    p0 = (g % 2) * 64
                    dma(POOL, WP[p0:p0 + 64, g // 2, p0:p0 + 64], w_pool[g])
                for k in range(8):
                    emit(DVE, "tensor_tensor", out=WO[:, k, :], in0=WOS[:, k, :], in1=GABC[:, :], op=ALU.mult)
                if dbg:
                    dma(SP, dbg_h[:, 0:48], MODC[:, :])
                    dma(SP, dbg_h[:, 48:64], AB[:, :])
                    dma(SP, dbg_h[:, 64:144], COLS[:, :])
                    dma(SP, dbg_h[:, 256:768], GABC[:, 0:512])
                cx.barrier()

            KN = cx.sb(es1, "kn", [128, 6, S], BF16)
            VT = cx.sb(es1, "vt", [128, 32, 768], BF16)
            KR = cx.sb(es1, "kr", [128, S], BF16)
            XT = [cx.sb(es1, f"xt{i}", [128, 1024], F32) for i in range(2)]
            XN = cx.sb(es1, "xn", [128, 1024], BF16)
            SS = cx.sb(es1, "ss", [128, 4], F32)
            HT = cx.sb(es1, "ht", [128, 8, 512], BF16)
            CAT = HT
            CQT = cx.sb(es1, "cqt", [128, 4, 512], BF16)
            CKVT = cx.sb(es1, "ckvt", [128, 2, 512], BF16)
            SQ = [cx.sb(es1, f"sq{i}", [128, 512], BF16) for i in range(2)]
            POOLED = cx.sb(es1, "pooled", [128, 2, 512], BF16)
            HALO = cx.sb(es1, "halo", [128, 2, 16], F32)
            COS2 = cx.sb(es1, "cos2", [64, 512], F32)
            SIN2 = cx.sb(es1, "sin2", [64, 512], F32)
            QN = [cx.sb(es1, f"qn{i}", [128, 512], BF16) for i in range(2)]
            QR = [cx.sb(es1, f"qr{i}", [128, 512], BF16) for i in range(2)]
            PT = [cx.sb(es1, f"pt{i}", [128, 512], BF16) for i in range(3)]
            RDEN = cx.sb(es1, "rden", [128, 512], F32)
            POSI = cx.sb(es1, "posi", [64, 512], I32)
            RKVC = cx.sb(es1, "rkvc", [128, 8], F32)
            P7B = PS[7].bitcast(BF16)

            emit(DVE, "memset", ap=HALO[:, :, :], constant=0.0)
            emit(POOL, "memset", ap=KR[64:128, :], constant=0.0)
            for q_ in QR:
                emit(POOL, "memset", ap=q_[64:128, :], constant=0.0)
            A1 = AB

            for j in range(nblk):
                for tt in range(4):
                    t = 4 * j + tt
                    xt = XT[t % 2]
                    dma(SP, xt[:, :], x[t * 128:(t + 1) * 128, :])
                    emit(ACT, "activation", out=XN[:, :], in_=xt[:, :], func=AF.Square, accum_out=SS[:, 0:1])
                    emit(ACT, "activation", out=SS[:, 1:2], in_=SS[:, 0:1], func=AF.Sqrt, scale=1.0 / D, bias=EPS)
                    emit(DVE, "reciprocal", out=SS[:, 2:3], in_=SS[:, 1:2])
                    emit(ACT, "activation", out=XN[:, :], in_=xt[:, :], func=AF.Copy, scale=SS[:, 2:3])
                    for k in range(8):
                        emit(PE, "transpose", out=P7B[:, k * 128:(k + 1) * 128], in_=XN[:, k * 128:(k + 1) * 128],
                             identity=IDB[:, :], inc=(k == 7))
                    for k in range(8):
                        emit(DVE, "tensor_scalar", out=HT[:, k, tt * 128:(tt + 1) * 128],
                             in0=P7B[:, k * 128:(k + 1) * 128], scalar1=A1[:, k:k + 1], scalar2=MODC[:, k:k + 1],
                             op0=ALU.mult, op1=ALU.add)

                if cut <= 1:
                    continue
                def proj(c0, c1):
                    ps = nextps()
                    c1 = c0 + 128
                    m = 128
                    for k in range(8):
                        emit(PE, "matmul", out=ps[0:m, :], lhsT=WIN[:, k, c0:c1], rhs=HT[:, k, :],
                             start=(k == 0), stop=(k == 7), inc=(k == 7))
                    return ps

                SCR0, SCR1 = XT[0], XT[1]
                POSF = SCR0[0:64, 0:512]
                ANG = SCR0[0:64, 512:1024]
                dma(SP, POSI[:, :], pos[0, j * 512:(j + 1) * 512].partition_broadcast(64))
                emit(DVE, "tensor_copy", out=POSF, in_=POSI[:, :])
                emit(DVE, "tensor_scalar", out=ANG, in0=POSF, scalar1=ROPEC[:, 0:1], scalar2=None, op0=ALU.mult)
                emit(DVE, "tensor_scalar", out=POSF, in0=ANG, scalar1=1.0 / TWO_PI, scalar2=None, op0=ALU.mult)
                emit(DVE, "tensor_copy", out=POSI[:, :], in_=POSF)
                emit(DVE, "tensor_copy", out=POSF, in_=POSI[:, :])
                emit(DVE, "scalar_tensor_tensor", out=ANG, in0=POSF, scalar=-TWO_PI, in1=ANG, op0=ALU.mult,
                     op1=ALU.add)
                emit(DVE, "tensor_scalar", out=POSF, in0=ANG, scalar1=math.pi, scalar2=None, op0=ALU.is_gt)
                emit(DVE, "scalar_tensor_tensor", out=ANG, in0=POSF, scalar=-TWO_PI, in1=ANG, op0=ALU.mult,
                     op1=ALU.add)
                emit(DVE, "tensor_scalar", out=POSF, in0=ANG, scalar1=-math.pi, scalar2=None, op0=ALU.is_lt)
                emit(DVE, "scalar_tensor_tensor", out=ANG, in0=POSF, scalar=TWO_PI, in1=ANG, op0=ALU.mult,
                     op1=ALU.add)
                emit(DVE, "tensor_scalar", out=POSF, in0=ANG, scalar1=math.pi / 2, scalar2=None, op0=ALU.is_gt)
                emit(DVE, "scalar_tensor_tensor", out=POSF, in0=POSF, scalar=-TWO_PI, in1=ANG, op0=ALU.mult,
                     op1=ALU.add)
                emit(DVE, "tensor_scalar", out=POSF, in0=POSF, scalar1=math.pi / 2, scalar2=None, op0=ALU.add)
                emit(DVE, "tensor_scalar", out=POSF, in0=POSF, scalar1=-3.14159, scalar2=3.14159, op0=ALU.max,
                     op1=ALU.min)
                emit(DVE, "tensor_scalar", out=ANG, in0=ANG, scalar1=-3.14159, scalar2=3.14159, op0=ALU.max,
                     op1=ALU.min)
                emit(ACT, "activation", out=COS2[:, :], in_=POSF, func=AF.Sin)
                emit(ACT, "activation", out=SIN2[:, :], in_=ANG, func=AF.Sin)
                emit(DVE, "tensor_scalar", out=SIN2[:, :], in0=SIN2[:, :], scalar1=ROPEC[:, 1:2], scalar2=None,
                     op0=ALU.mult)

                for c in range(2):
                    ps = proj(c * 128, (c + 1) * 128)
                    for hh in range(2):
                        PB = SCR0[:, 0:272]
                        T1 = SCR0[:, 272:544]
                        T2 = SCR0[:, 544:816]
                        TM = SCR0[:, 816:832]
                        emit(DVE, "tensor_copy", out=PB[:, 0:16], in_=HALO[:, c, :])
                        emit(DVE, "tensor_copy", out=PB[:, 16:272], in_=ps[:, hh * 256:(hh + 1) * 256])
                        emit(DVE, "tensor_copy", out=HALO[:, c, :], in_=PB[:, 256:272])
                        emit(DVE, "tensor_tensor", out=T1[:, 1:272], in0=PB[:, 1:272], in1=PB[:, 0:271], op=ALU.add)
                        if c == 0:
                            emit(DVE, "tensor_tensor", out=T2[64:128, 3:272], in0=T1[64:128, 3:272],
                                 in1=T1[64:128, 1:270], op=ALU.add)
                        else:
                            emit(DVE, "tensor_tensor", out=T2[:, 3:272], in0=T1[:, 3:272], in1=T1[:, 1:270],
                                 op=ALU.add)
                            emit(DVE, "tensor_tensor", out=T1[:, 7:272], in0=T2[:, 7:272], in1=T2[:, 3:268],
                                 op=ALU.add)
                            emit(DVE, "tensor_tensor", out=T2[64:128, 15:272], in0=T1[64:128, 15:272],
                                 in1=T1[64:128, 7:264], op=ALU.add)
                        for (p0, p1, src) in ((0, 64, T1), (64, 128, T2)):
                            emit(DVE, "scalar_tensor_tensor", out=POOLED[p0:p1, c, hh * 256:(hh + 1) * 256],
                                 in0=src[p0:p1, 16:272], scalar=POOLC[p0:p1, c * 17:c * 17 + 1],
                                 in1=PB[p0:p1, 16:272], op0=ALU.mult, op1=ALU.subtract)
                            if j == 0 and hh == 0:
                                emit(DVE, "tensor_tensor", out=TM[p0:p1, :], in0=src[p0:p1, 16:32],
                                     in1=POOLC[p0:p1, c * 17 + 1:c * 17 + 17], op=ALU.mult)
                                emit(DVE, "tensor_tensor", out=POOLED[p0:p1, c, 0:16], in0=TM[p0:p1, :],
                                     in1=PB[p0:p1, 16:32], op=ALU.subtract)

                for i in range(4):
                    ps = proj(256 + i * 128, 256 + (i + 1) * 128)
                    emit(ACT, "activation", out=CQT[:, i, :], in_=ps[:, :], func=AF.Copy, scale=COLS[:, 74 + i:75 + i])
                    emit(ACT, "activation", out=SQ[i % 2][:, :], in_=ps[:, :], func=AF.Square)
                    emit(PE, "matmul", out=PS[3][:, :], lhsT=ONESB[:, :], rhs=SQ[i % 2][:, :], start=(i == 0),
                         stop=(i == 3))
                for i in range(2):
                    ps = proj(768 + i * 128, 768 + (i + 1) * 128)
                    emit(ACT, "activation", out=CKVT[:, i, :], in_=ps[:, :], func=AF.Copy, scale=COLS[:, 78 + i:79 + i])
                    emit(ACT, "activation", out=SQ[i][:, :], in_=ps[:, :], func=AF.Square)
                for i in range(2):
                    emit(PE, "matmul", out=PS[4][:, :], lhsT=ONESB[:, :], rhs=SQ[i][:, :], start=(i == 0), stop=(i == 1))
                for tt in range(4):
                    for i in range(2):
                        emit(PE, "matmul", out=PS[5][:, tt:tt + 1], lhsT=SQ[i][:, tt * 128:(tt + 1) * 128],
                             rhs=ONESB[:, 0:1], start=(i == 0), stop=(i == 1), inc=(i == 1))
                RQ = SCR1[:, 0:512]
                RKV = SCR1[:, 512:1024]
                emit(ACT, "activation", out=RQ, in_=PS[3][:, :], func=AF.Sqrt, scale=1.0 / 512, bias=EPS)
                emit(DVE, "reciprocal", out=RQ, in_=RQ)
                emit(DVE, "tensor_scalar", out=RQ, in0=RQ, scalar1=SM_SCALE, scalar2=None, op0=ALU.mult)
                emit(ACT, "activation", out=RKV, in_=PS[4][:, :], func=AF.Sqrt, scale=1.0 / 256, bias=EPS)
                emit(DVE, "reciprocal", out=RKV, in_=RKV)
                emit(ACT, "activation", out=RKVC[:, 0:4], in_=PS[5][:, 0:4], func=AF.Sqrt, scale=1.0 / 256, bias=EPS)
                emit(DVE, "reciprocal", out=RKVC[:, 4:8], in_=RKVC[:, 0:4])

                ps1 = proj(1024, 1088)
                ps2 = proj(1088, 1152)
                TA = SCR0[0:64, 0:512]
                TB = SCR0[0:64, 512:1024]
                emit(DVE, "tensor_tensor", out=TA, in0=ps1[0:64, :], in1=COS2[:, :], op=ALU.mult)
                emit(DVE, "tensor_tensor", out=TB, in0=ps2[0:64, :], in1=SIN2[:, :], op=ALU.mult)
                emit(DVE, "tensor_tensor", out=KR[0:64, j * 512:(j + 1) * 512], in0=TA, in1=TB, op=ALU.add)

                for h in range(6):
                    ps = nextps()
                    for k in range(2):
                        emit(PE, "matmul", out=ps[:, :], lhsT=WUKV[:, k, h * 256:h * 256 + 128], rhs=CKVT[:, k, :],
                             start=(k == 0), stop=(k == 1), inc=(k == 1))
                    emit(DVE, "tensor_tensor", out=KN[:, h, j * 512:(j + 1) * 512], in0=ps[:, :], in1=RKV,
                         op=ALU.mult)
                for tt in range(4):
                    t = 4 * j + tt
                    for half in range(2):
                        ps = nextps()
                        for k in range(2):
                            rhs = WUKV[:, k, :].rearrange("p (h t d) -> p h t d", t=2, d=128)[:, half * 3:half * 3 + 3, 1, :]
                            emit(PE, "matmul", out=ps[:, 0:384].rearrange("p (h d) -> p h d", d=128),
                                 lhsT=CKVT[:, k, tt * 128:(tt + 1) * 128], rhs=rhs, start=(k == 0), stop=(k == 1),
                                 inc=(k == 1))
                        emit(ACT, "activation", out=VT[:, t, half * 384:(half + 1) * 384], in_=ps[:, 0:384],
                             func=AF.Copy, scale=RKVC[:, 4 + tt:5 + tt])

                if cut <= 2:
                    continue
                def qproj(h):
                    ps = nextps()
                    for k in range(4):
                        emit(PE, "matmul", out=ps[:, :], lhsT=WUQ[:, k, h * 192:h * 192 + 128], rhs=CQT[:, k, :],
                             start=(k == 0), stop=(k == 3), inc=(k == 3))
                    emit(DVE, "tensor_tensor", out=QN[h % 2][:, :], in0=ps[:, :], in1=RQ, op=ALU.mult)
                    pa = nextps()
                    for k in range(4):
                        emit(PE, "matmul", out=pa[:, :], lhsT=WUQ[:, k, h * 192 + 128:h * 192 + 256],
                             rhs=CQT[:, k, :], start=(k == 0), stop=(k == 3), inc=(k == 3))
                    pb = nextps()
                    for k in range(4):
                        emit(PE, "matmul", out=pb[:, :], lhsT=WUQ[:, k, 1152 + h * 64:1152 + h * 64 + 128],
                             rhs=CQT[:, k, :], start=(k == 0), stop=(k == 3), inc=(k == 3))
                    emit(DVE, "tensor_tensor", out=TA, in0=pa[0:64, :], in1=COS2[:, :], op=ALU.mult)
                    emit(DVE, "tensor_tensor", out=TB, in0=pb[0:64, :], in1=SIN2[:, :], op=ALU.mult)
                    emit(DVE, "tensor_tensor", out=TA, in0=TA, in1=TB, op=ALU.add)
                    emit(DVE, "tensor_tensor", out=QR[h % 2][0:64, :], in0=TA, in1=SCR1[0:64, 0:512], op=ALU.mult)

                nk = 4 * j + 4
                pcnt = [0]

                def s_tile(h, i):
                    r = i - 4 * j
                    qo = 128 * r if r > 0 else 0
                    n = 512 - qo
                    ps = nextps()
                    emit(PE, "matmul", out=ps[:, 0:n], lhsT=KN[:, h, i * 128:(i + 1) * 128], rhs=QN[h % 2][:, qo:512],
                         start=True, stop=False, inc=False)
                    emit(PE, "matmul", out=ps[:, 0:n], lhsT=KR[:, i * 128:(i + 1) * 128], rhs=QR[h % 2][:, qo:512],
                         start=False, stop=True)
                    return ps, qo, n, r

                def attention(h, prev_final):
                    OB = PS[3] if h % 2 == 0 else PS[5]
                    DB = PS[4] if h % 2 == 0 else PS[6]
                    queue = [s_tile(h, i) for i in range(min(2, nk))]
                    for i in range(nk):
                        if i + 2 < nk:
                            queue.append(s_tile(h, i + 2))
                        ps, qo, n, r = queue.pop(0)
                        pt = PT[pcnt[0] % 3]
                        pcnt[0] += 1
                        emit(ACT, "activation", out=pt[:, 0:n], in_=ps[:, 0:n], func=AF.Exp)
                        if r >= 0:
                            emit(POOL, "tensor_tensor", out=pt[:, 0:128], in0=pt[:, 0:128], in1=TRI[:, :], op=ALU.mult)
                        emit(PE, "matmul", out=OB[:, qo:512], lhsT=VT[:, i, h * 128:(h + 1) * 128], rhs=pt[:, 0:n],
                             start=(i == 0), stop=(i == nk - 1))
                        if i == 0:
                            emit(DVE, "tensor_copy", out=DB[:, :], in_=pt[:, :])
                        else:
                            emit(DVE, "tensor_tensor", out=DB[:, qo:512], in0=DB[:, qo:512], in1=pt[:, 0:n], op=ALU.add)
                        if i == 1 and prev_final is not None:
                            prev_final()
                            prev_final = None
                    if prev_final is not None:
                        prev_final()

                    def final():
                        emit(DVE, "tensor_copy", out=SQ[0][:, :], in_=DB[:, :])
                        pd = nextps()
                        emit(PE, "matmul", out=pd[:, :], lhsT=ONESB[:, :], rhs=SQ[0][:, :], start=True, stop=True)
                        emit(DVE, "reciprocal", out=RDEN[:, :], in_=pd[:, :])
                        emit(DVE, "tensor_tensor", out=CAT[:, 2 + h, :], in0=OB[:, :], in1=RDEN[:, :], op=ALU.mult)
                    return final

                qproj(0)
                fin = None
                for h in range(6):
                    if h + 1 < 6:
                        qproj(h + 1)
                    fin = attention(h, fin)
                fin()

                for c in range(2):
                    ps = nextps()
                    emit(PE, "matmul", out=ps[:, :], lhsT=WP[:, c, :], rhs=POOLED[:, c, :], start=True, stop=True)
                    emit(ACT, "activation", out=CAT[:, c, :], in_=ps[:, :], func=AF.Copy, scale=COLS[:, 72 + c:73 + c])

                if cut <= 3:
                    continue
                for tt in range(4):
                    t = 4 * j + tt
                    xr = XT[tt % 2]
                    dma(SP, xr[:, :], x[t * 128:(t + 1) * 128, :])
                    for dh in range(2):
                        ps = nextps()
                        for fc in range(8):
                            emit(PE, "matmul", out=ps[:, :], lhsT=CAT[:, fc, tt * 128:(tt + 1) * 128],
                                 rhs=WO[:, fc, dh * 512:(dh + 1) * 512], start=(fc == 0), stop=(fc == 7),
                                 inc=(fc == 7))
                        emit(DVE, "tensor_tensor", out=xr[:, dh * 512:(dh + 1) * 512], in0=ps[:, :],
                             in1=xr[:, dh * 512:(dh + 1) * 512], op=ALU.add)
                    dma(SP, x1s[t], xr[:, :])
                    if stage == 1:
                        dma(SP, yv[t], xr[:, :])
            cx.barrier()

        if stage >= 2:
            es2 = contextlib.ExitStack()
            with es2:
                NS = 4
                WGU = [cx.sb(es2, f"wgu{i}", [128, 8, 512], BF16) for i in range(2)]
                WD = [cx.sb(es2, f"wd{i}", [128, 2, 1024], BF16) for i in range(2)]
                H2T = [cx.sb(es2, f"h2t{i}", [128, 8, 1024], BF16) for i in range(2)]
                ACC = [[cx.sb(es2, f"acc{i}_{d_}", [128, 8, 512], F32) for d_ in range(2)] for i in range(2)]
                YS = [cx.sb(es2, f"ys{i}", [128, 512], F32) for i in range(2)]
                WT = [cx.sb(es2, f"wt{i}", [128, 8, 32], F32) for i in range(2)]
                XT2 = [cx.sb(es2, f"xm{i}", [128, 1024], F32) for i in range(2)]
                XE = [cx.sb(es2, f"xe{i}", [128, 1024], F32) for i in range(2)]
                XNF = cx.sb(es2, "xnf", [128, 1024], F32)
                H2F = cx.sb(es2, "h2f", [128, 8, 128], F32)
                SG = [cx.sb(es2, f"sg{i}", [128, 512], F32) for i in range(2)]
                AT = [cx.sb(es2, f"at{i}", [128, 2, 512], BF16) for i in range(2)]
                GFBC = cx.sb(es2, "gfbc", [128, 1024], F32)
                FGBC = cx.sb(es2, "fgbc", [128, 1024], F32)
                WGR = cx.sb(es2, "wgr", [128, 8, 36], F32)
                BGR = cx.sb(es2, "bgr", [128, 36], F32)
                LG = cx.sb(es2, "lg", [128, 36], F32)
                RT = cx.sb(es2, "rt", [128, 128], F32)
                SS2 = cx.sb(es2, "ss2", [128, 4], F32)
                SS3 = cx.sb(es2, "ss3", [128, 4], F32)
                YT = [cx.sb(es2, f"yt{i}", [128, 1024], F32) for i in range(2)]
                JUNK = cx.sb(es2, "junk", [128, 1024], BF16)
                JUNK2 = cx.sb(es2, "junk2", [128, 1024], BF16)

                dma(SP, GFBC[:, :], V(grow_h[0, 1024:2048].partition_broadcast(128), grow_b))
                dma(SP, FGBC[:, :], final_g.partition_broadcast(128))
                dma(SP, WGR[:, :, :], w_gr.rearrange("(k p) n -> p k n", p=128))
                dma(SP, BGR[:, :], b_gr.partition_broadcast(128))
                A2 = AB[:, 8:16]
                PP = PS[7]

                def load_expert(ge):
                    e = ge % NEXP
                    dma(POOL, WGU[ge % 2][:, :, :], w_gu[e].rearrange("(k p) n -> p k n", p=128))
                    dma(POOL, WD[ge % 2][:, :, :], w_dn[e].rearrange("(k p) n -> p k n", p=128))

                pro_cnt = [0]

                def prologue_stages(sb_, tl):
                    t = sb_ * 8 + tl
                    h2t, wt = H2T[sb_ % 2], WT[sb_ % 2]
                    def st0():
                        xt = XT2[pro_cnt[0] % 2]
                        pro_cnt[0] += 1
                        dma(SP, xt[:, :], x1s[t])
                        emit(ACT, "activation", out=JUNK[:, :], in_=xt[:, :], func=AF.Square, accum_out=SS2[:, 0:1])
                        emit(ACT, "activation", out=SS2[:, 1:2], in_=SS2[:, 0:1], func=AF.Sqrt, scale=1.0 / D, bias=EPS)
                        emit(DVE, "reciprocal", out=SS2[:, 2:3], in_=SS2[:, 1:2])
                        emit(ACT, "activation", out=XNF[:, :], in_=xt[:, :], func=AF.Copy, scale=SS2[:, 2:3])
                    def st1():
                        for half in (0,):
                            for kk in range(4):
                                k = half * 4 + kk
                                emit(PE, "transpose", out=PP[:, kk * 128:(kk + 1) * 128],
                                     in_=XNF[:, k * 128:(k + 1) * 128], identity=IDF[:, :], inc=(kk == 3))
                            for kk in range(4):
                                k = half * 4 + kk
                                emit(DVE, "tensor_scalar", out=H2F[:, k, :], in0=PP[:, kk * 128:(kk + 1) * 128],
                                     scalar1=A2[:, k:k + 1], scalar2=MODC[:, 24 + k:25 + k], op0=ALU.mult,
                                     op1=ALU.add)
                    def st2():
                        for half in (1,):
                            for kk in range(4):
                                k = half * 4 + kk
                                emit(PE, "transpose", out=PP[:, kk * 128:(kk + 1) * 128],
                                     in_=XNF[:, k * 128:(k + 1) * 128], identity=IDF[:, :], inc=(kk == 3))
                            for kk in range(4):
                                k = half * 4 + kk
                                emit(DVE, "tensor_scalar", out=H2F[:, k, :], in0=PP[:, kk * 128:(kk + 1) * 128],
                                     scalar1=A2[:, k:k + 1], scalar2=MODC[:, 24 + k:25 + k], op0=ALU.mult,
                                     op1=ALU.add)
                        emit(ACT, "activation", out=h2t[:, :, tl * 128:(tl + 1) * 128], in_=H2F[:, :, :], func=AF.Copy)
                    def st3():
                        for k in range(8):
                            emit(PE, "matmul", out=PP[:, 0:36], lhsT=H2F[:, k, :], rhs=WGR[:, k, :], start=(k == 0),
                                 stop=(k == 7), inc=(k == 7))
                        emit(DVE, "tensor_copy", out=LG[:, :], in_=PP[:, 0:36])
                        GLB = RT[:, 0:4]
                        GM = RT[:, 4:5]
                        GOH = RT[:, 8:12]
                        GEX = RT[:, 12:16]
                        GS = RT[:, 16:17]
                        GP = RT[:, 17:18]
                        GM2 = RT[:, 18:19]
                        EIN = RT[:, 24:32]
                        ZB = RT[:, 32:40]
                        M1 = RT[:, 40:41]
                        OH1 = RT[:, 48:56]
                        OH2 = RT[:, 56:64]
                        PEX = RT[:, 64:72]
                        DEN = RT[:, 72:73]
                        WL = RT[:, 80:88]
                        emit(DVE, "tensor_tensor", out=GLB, in0=LG[:, 0:4], in1=BGR[:, 0:4], op=ALU.add)
                        emit(DVE, "tensor_reduce", out=GM, in_=GLB, axis=AX.X, op=ALU.max)
                        emit(DVE, "tensor_scalar", out=GOH, in0=GLB, scalar1=GM, scalar2=None, op0=ALU.is_ge)
                        emit(DVE, "tensor_reduce", out=GM2, in_=LG[:, 0:4], axis=AX.X, op=ALU.max)
                        emit(DVE, "tensor_scalar", out=GEX, in0=LG[:, 0:4], scalar1=GM2, scalar2=None, op0=ALU.subtract)
                        emit(ACT, "activation", out=GEX, in_=GEX, func=AF.Exp)
                        emit(DVE, "tensor_reduce", out=GS, in_=GEX, axis=AX.X, op=ALU.add)
                        emit(DVE, "tensor_tensor", out=GEX, in0=GEX, in1=GOH, op=ALU.mult)
                        emit(DVE, "tensor_reduce", out=GP, in_=GEX, axis=AX.X, op=ALU.add)
                        emit(DVE, "reciprocal", out=GS, in_=GS)
                        emit(DVE, "tensor_tensor", out=GP, in0=GP, in1=GS, op=ALU.mult)
                        emit(DVE, "tensor_scalar", out=EIN, in0=LG[:, 4:12], scalar1=RT[:, 8:9], scalar2=None, op0=ALU.mult)
                        emit(DVE, "tensor_scalar", out=ZB, in0=BGR[:, 4:12], scalar1=RT[:, 8:9], scalar2=None, op0=ALU.mult)
                        for g in range(1, 4):
                            emit(DVE, "scalar_tensor_tensor", out=EIN, in0=LG[:, 4 + 8 * g:12 + 8 * g],
                                 scalar=RT[:, 8 + g:9 + g], in1=EIN, op0=ALU.mult, op1=ALU.add)
                            emit(DVE, "scalar_tensor_tensor", out=ZB, in0=BGR[:, 4 + 8 * g:12 + 8 * g],
                                 scalar=RT[:, 8 + g:9 + g], in1=ZB, op0=ALU.mult, op1=ALU.add)
                        emit(DVE, "tensor_tensor", out=ZB, in0=ZB, in1=EIN, op=ALU.add)
                        emit(DVE, "tensor_reduce", out=M1, in_=ZB, axis=AX.X, op=ALU.max)
                        emit(DVE, "tensor_scalar", out=OH1, in0=ZB, scalar1=M1, scalar2=None, op0=ALU.is_ge)
                        emit(DVE, "scalar_tensor_tensor", out=ZB, in0=OH1, scalar=-1e30, in1=ZB, op0=ALU.mult, op1=ALU.add)
                        emit(DVE, "tensor_reduce", out=M1, in_=ZB, axis=AX.X, op=ALU.max)
                        emit(DVE, "tensor_scalar", out=OH2, in0=ZB, scalar1=M1, scalar2=None, op0=ALU.is_ge)
                        emit(DVE, "tensor_tensor", out=OH1, in0=OH1, in1=OH2, op=ALU.add)
                        emit(DVE, "tensor_reduce", out=M1, in_=EIN, axis=AX.X, op=ALU.max)
                        emit(DVE, "tensor_scalar", out=PEX, in0=EIN, scalar1=M1, scalar2=None, op0=ALU.subtract)
                        emit(ACT, "activation", out=PEX, in_=PEX, func=AF.Exp)
                        emit(DVE, "tensor_tensor", out=PEX, in0=PEX, in1=OH1, op=ALU.mult)
                        emit(DVE, "tensor_reduce", out=DEN, in_=PEX, axis=AX.X, op=ALU.add)
                        emit(DVE, "reciprocal", out=DEN, in_=DEN)
                        emit(DVE, "tensor_tensor", out=DEN, in0=DEN, in1=GP, op=ALU.mult)
                        emit(DVE, "tensor_scalar", out=WL, in0=PEX, scalar1=DEN, scalar2=None, op0=ALU.mult)
                        for g in range(4):
                            emit(DVE, "tensor_scalar", out=wt[:, tl, g * 8:(g + 1) * 8], in0=WL,
                                 scalar1=RT[:, 8 + g:9 + g], scalar2=None, op0=ALU.mult)

                    return [st0, st1, st2, st3]

                def prologue_tile(sb_, tl):
                    for f_ in prologue_stages(sb_, tl):
                        f_()

                epi_cnt = [0]

                def epilogue_stages(sb_, tl):
                    t = sb_ * 8 + tl
                    acc = ACC[sb_ % 2]
                    xt = XE[epi_cnt[0] % 2]
                    yt = YT[epi_cnt[0] % 2]
                    epi_cnt[0] += 1

                    def ea():
                        dma(SP, xt[:, :], x1s[t])
                        for d_ in range(2):
                            emit(POOL, "tensor_tensor", out=acc[d_][:, tl, :], in0=acc[d_][:, tl, :],
                                 in1=GFBC[:, d_ * 512:(d_ + 1) * 512], op=ALU.mult)
                            emit(POOL, "tensor_tensor", out=xt[:, d_ * 512:(d_ + 1) * 512],
                                 in0=xt[:, d_ * 512:(d_ + 1) * 512], in1=acc[d_][:, tl, :], op=ALU.add)

                    def eb():
                        emit(ACT, "activation", out=JUNK2[:, :], in_=xt[:, :], func=AF.Square, accum_out=SS3[:, 0:1])
                        emit(ACT, "activation", out=SS3[:, 1:2], in_=SS3[:, 0:1], func=AF.Sqrt, scale=1.0 / D, bias=EPS)
                        emit(DVE, "reciprocal", out=SS3[:, 2:3], in_=SS3[:, 1:2])
                        emit(DVE, "scalar_tensor_tensor", out=yt[:, :], in0=xt[:, :], scalar=SS3[:, 2:3],
                             in1=FGBC[:, :], op0=ALU.mult, op1=ALU.mult)
                        dma(SP, yv[t], yt[:, :])
                    return ea, eb

                def epilogue_tile(sb_, tl):
                    ea, eb = epilogue_stages(sb_, tl)
                    ea()
                    eb()

                dcnt = [0]
                ycnt = [0]

                def gu_unit(sb_, e, sb):
                    ge = sb_ * NEXP + e
                    wgu = WGU[ge % 2]
                    h2t = H2T[sb_ % 2]
                    at = AT[(ge * 2 + sb) % 2]
                    for fc in range(2):
                        pg = PS[fc * 2]
                        pu = PS[fc * 2 + 1]
                        for k in range(8):
                            emit(PE, "matmul", out=pg[:, :], lhsT=wgu[:, k, fc * 128:(fc + 1) * 128],
                                 rhs=h2t[:, k, sb * 512:(sb + 1) * 512], start=(k == 0), stop=(k == 7),
                                 inc=(k == 7))
                        for k in range(8):
                            emit(PE, "matmul", out=pu[:, :], lhsT=wgu[:, k, 256 + fc * 128:256 + (fc + 1) * 128],
                                 rhs=h2t[:, k, sb * 512:(sb + 1) * 512], start=(k == 0), stop=(k == 7),
                                 inc=(k == 7))
                        sg = SG[fc]
                        emit(ACT, "activation", out=sg[:, :], in_=pg[:, :], func=AF.Silu)
                        emit(DVE, "tensor_tensor", out=at[:, fc, :], in0=pu[:, :], in1=sg[:, :], op=ALU.mult)

                def dn_unit(sb_, e, sb):
                    ge = sb_ * NEXP + e
                    wd = WD[ge % 2]
                    acc, wt = ACC[sb_ % 2], WT[sb_ % 2]
                    at = AT[(ge * 2 + sb) % 2]
                    for tt in range(4):
                        tl = sb * 4 + tt
                        for dh in range(2):
                            po = PS[4 + dcnt[0] % 3]
                            dcnt[0] += 1
                            for fc in range(2):
                                emit(PE, "matmul", out=po[:, :], lhsT=at[:, fc, tt * 128:(tt + 1) * 128],
                                     rhs=wd[:, fc, dh * 512:(dh + 1) * 512], start=(fc == 0), stop=(fc == 1),
                                     inc=(fc == 1))
                            accv = acc[dh][:, tl, :]
                            if e == 0:
                                emit(DVE, "tensor_scalar", out=accv, in0=po[:, :], scalar1=wt[:, tl, e:e + 1],
                                     scalar2=None, op0=ALU.mult)
                            else:
                                emit(DVE, "scalar_tensor_tensor", out=accv, in0=po[:, :],
                                     scalar=wt[:, tl, e:e + 1], in1=accv, op0=ALU.mult, op1=ALU.add)

                load_expert(0)
                load_expert(1)
                for tl in range(8):
                    prologue_tile(0, tl)
                units = [(sb_, e, sb) for sb_ in range(NS) for e in range(NEXP) for sb in range(2)]
                gu_unit(*units[0])
                stage_q = []
                for ui, (sb_, e, sb) in enumerate(units):
                    if ui + 1 < len(units):
                        gu_unit(*units[ui + 1])
                    dn_unit(sb_, e, sb)
                    uu = e * 2 + sb
                    if uu == 0 and sb_ + 1 < NS:
                        stage_q = [f_ for k in range(8) for f_ in prologue_stages(sb_ + 1, k)]
                    if uu % 2 == 0 and uu // 2 < len(stage_q):
                        stage_q[uu // 2]()
                    if sb == 1:
                        ge = sb_ * NEXP + e
                        if ge + 2 < NS * NEXP:
                            load_expert(ge + 2)
                        if e % 4 == 1 and sb_ >= 1:
                            epi_pending = epilogue_stages(sb_ - 1, e // 4)
                            epi_pending[0]()
                        if e % 4 == 2 and sb_ >= 1:
                            epi_pending[1]()
                    if uu == 63:
                        stage_q = []
                for tl in range(8):
                    epilogue_tile(NS - 1, tl)

        for v in yv:
            for s, val in v.buf.w.values():
                SP.wait_tok(s, val)
        cx.barrier()
        if dbg:
            print("SEMCOUNTS", {E.name: E.n for E in cx.engs}, "dma max", max(c_ for _, c_ in cx.dma_sems), "nsem", cx.nsem)
    return nc


_CACHE = {}


def _consts():
    ident = np.eye(128, dtype=np.float32)
    tri = (np.arange(128)[None, :] >= np.arange(128)[:, None]).astype(np.float32)
    inv_freq = (10000.0 ** (-(np.arange(0, 64, 2, dtype=np.float32) / np.float32(64)))).astype(np.float32)
    ropec = np.zeros((64, 2), np.float32)
    ropec[:, 0] = np.concatenate([inv_freq, inv_freq])
    ropec[:32, 1] = -1.0
    ropec[32:, 1] = 1.0
    poolc = np.zeros((128, 2, 17), np.float32)
    wins = (2, 4, 8, 16)
    for c in range(2):
        for half in range(2):
            w = wins[c * 2 + half]
            sl = slice(half * 64, half * 64 + 64)
            poolc[sl, c, 0] = 1.0 / w
            poolc[sl, c, 1:17] = 1.0 / np.minimum(np.arange(1, 17), w)
    return dict(ident_bf=ident.astype(ml_dtypes.bfloat16), ident_f=ident, tri=tri.astype(ml_dtypes.bfloat16),
                ropec=ropec, poolc=poolc.reshape(128, 34))


def make_in_maps(inputs, cores):
    cst = _consts()
    f = lambda a: np.ascontiguousarray(a, dtype=np.float32)
    shared = dict(
        bmod_row=f(inputs["b_mod"][0]).reshape(1, 6144), final_g=f(inputs["final_g"]),
        b_gr=f(np.concatenate([inputs["b_group"][0], inputs["b_router"][0]])),
        w_mod=f(inputs["w_mod"][0]), w_in=f(inputs["w_in"][0]), w_pool=f(inputs["w_pool"][0]),
        w_uq=f(inputs["w_uq"][0]), w_ukv=f(inputs["w_ukv"][0]), w_o=f(inputs["w_o"][0]),
        w_gr=f(np.concatenate([inputs["w_group"][0], inputs["w_router"][0]], axis=1)),
        w_gate_up=f(inputs["w_gate_up"][0]), w_down=f(inputs["w_down"][0]), **cst)
    maps = []
    for b in cores:
        small = np.concatenate([
            f(inputs["b_mod"][0]).reshape(48, 128), f(inputs["c"][b]).reshape(8, 128),
            f(inputs["norm_mix_g"][0]).reshape(8, 128), f(inputs["norm_ffn_g"][0]).reshape(8, 128),
            f(inputs["pool_scale"][0]).reshape(2, 128), f(inputs["q_norm_g"][0]).reshape(4, 128),
            f(inputs["kv_norm_g"][0]).reshape(2, 128)], axis=0)
        m = dict(shared)
        m["x"] = f(inputs["x"][b])
        m["pos"] = np.ascontiguousarray(inputs["positions"][b], dtype=np.int32).reshape(1, S)
        m["small"] = np.ascontiguousarray(small)
        maps.append(m)
    return maps


def kernel(**inputs):
    inputs = {k: np.asarray(v) for k, v in inputs.items()}
    if "nc" not in _CACHE:
        _CACHE["nc"] = build(stage=2)
    nc = _CACHE["nc"]
    maps = make_in_maps(inputs, list(range(8)))
    res = run_bass_kernel_spmd(nc, maps, core_ids=list(range(8)))
    out = np.stack([np.asarray(r["y"], dtype=np.float32) for r in res.results], axis=0)
    return out
```
